# Optimizing a Trainium2 kernel written in Bass

```python
import jax, jax.numpy as jnp
from jax import lax
import numpy as np

D_MODEL = 1024
BATCH = 8
SEQ = 4096
DEPTH = 2

GRID_W = 64
N_GROUPS = 4
GROUP_W = D_MODEL // N_GROUPS
NA_HEADS = 4
NA_HEAD_DIM = GROUP_W // NA_HEADS
WIN_H = 8
WIN_W = 16
RWKV_HEADS = 4
RWKV_HEAD_DIM = GROUP_W // RWKV_HEADS
DECAY_LORA = 32
AAA_LORA = 32
GATE_LORA = 64
LN_X_EPS = 64e-5
POOL_GROUPS = 4
POOL_CH = GROUP_W // POOL_GROUPS
POOL_WINDOWS = (2, 4, 8, 16)
CONV_K = 3
FFN_DIM = 7 * D_MODEL // 2
N_EXPERTS = 8
TOP_K = 2
EXPERT_FFN = 7 * D_MODEL // 2
MOE_BLOCK = 256
N_DENSE = (DEPTH + 1) // 2
N_MOE = DEPTH // 2
RMS_EPS = 1e-6
RW_SHIFT_W = 3 * GROUP_W + DECAY_LORA + AAA_LORA
IN_W = 3 * GROUP_W + RW_SHIFT_W + GATE_LORA + GROUP_W + 3 * GROUP_W

kernel_name = 'hybrid_parallel_groups_encoder'


def rms_norm(x, g):
    xf = x.astype(jnp.float32)
    y = xf * lax.rsqrt(jnp.mean(xf * xf, axis=-1, keepdims=True) + RMS_EPS)
    return (y * g.astype(jnp.float32)).astype(x.dtype)


def neighbourhood_attention(q, k, v, rpb):
    bsz, t, _ = q.shape
    rows = t // GRID_W
    kh = min(WIN_H, rows)

    def to_grid(u):
        return u.reshape(bsz, rows, GRID_W, NA_HEADS, NA_HEAD_DIM).transpose(1, 0, 3, 2, 4)

    qg = to_grid(q) * (NA_HEAD_DIM ** -0.5)
    kg = to_grid(k)
    vg = to_grid(v)
    col = jnp.arange(GRID_W)
    col_start = jnp.clip(col - WIN_W // 2, 0, GRID_W - WIN_W)
    col_idx = col_start[:, None] + jnp.arange(WIN_W)[None, :]
    col_rel = col_idx - col[:, None] + (WIN_W - 1)
    row = jnp.arange(rows)
    row_start = jnp.clip(row - kh // 2, 0, rows - kh)

    def one_row(args):
        r, rs, q_r = args
        k_nb = lax.dynamic_slice_in_dim(kg, rs, kh, axis=0)[:, :, :, col_idx]
        v_nb = lax.dynamic_slice_in_dim(vg, rs, kh, axis=0)[:, :, :, col_idx]
        s = jnp.einsum('bhqd,ibhqjd->bhqij', q_r, k_nb).astype(jnp.float32)
        row_rel = rs + jnp.arange(kh) - r + (WIN_H - 1)
        bias = rpb[:, row_rel][:, :, col_rel]
        s = s + jnp.transpose(bias, (0, 2, 1, 3)).astype(jnp.float32)[None]
        p = jax.nn.softmax(s.reshape(bsz, NA_HEADS, GRID_W, kh * WIN_W), axis=-1)
        p = p.reshape(bsz, NA_HEADS, GRID_W, kh, WIN_W).astype(v.dtype)
        return jnp.einsum('bhqij,ibhqjd->bhqd', p, v_nb)

    out = lax.map(one_row, (row, row_start, qg))
    return out.transpose(1, 0, 3, 2, 4).reshape(bsz, t, NA_HEADS * NA_HEAD_DIM)


def rwkv7_step(S, inp):
    r, w, k, v, a_vec, b_vec = inp
    sa = jnp.einsum('...ij,...j->...i', S, a_vec)
    S = S * w[..., None, :] + sa[..., :, None] * b_vec[..., None, :] + v[..., :, None] * k[..., None, :]
    return S, jnp.einsum('...ij,...j->...i', S, r)


def rwkv7_bidirectional(zr, lg, mu, w0, w2, a0, a2, k_k, k_a, r_k, lnx_w, lnx_b, g2):
    bsz, t, _ = zr.shape
    G, H, N = GROUP_W, RWKV_HEADS, RWKV_HEAD_DIM
    z_prev = jnp.pad(zr, ((0, 0), (1, 0), (0, 0)))[:, :-1]
    z_next = jnp.pad(zr, ((0, 0), (0, 1), (0, 0)))[:, 1:]
    zd = zr[None] + mu[:, None, None, :] * (jnp.stack([z_prev, z_next]) - zr[None])
    r = zd[..., :G]
    k = zd[..., G:2 * G]
    v = zd[..., 2 * G:3 * G]
    lw = zd[..., 3 * G:3 * G + DECAY_LORA]
    la = zd[..., 3 * G + DECAY_LORA:]
    w = -jax.nn.softplus(-(w0[:, None, None, :] + jnp.einsum('ebtl,elc->ebtc', jnp.tanh(lw), w2))) - 0.5
    decay = jnp.exp(-jnp.exp(w.astype(jnp.float32)))
    a = jax.nn.sigmoid(a0[:, None, None, :] + jnp.einsum('ebtl,elc->ebtc', la, a2))

    def heads(u):
        return u.astype(jnp.float32).reshape(2, bsz, t, H, N)

    r, k, v, decay, a = heads(r), heads(k), heads(v), heads(decay), heads(a)
    kk = k * k_k.astype(jnp.float32).reshape(H, N)
    kk = kk * lax.rsqrt(jnp.maximum(jnp.sum(kk * kk, axis=-1, keepdims=True), 1e-12))
    k = k * (1.0 + (a - 1.0) * k_a.astype(jnp.float32).reshape(H, N))

    def to_time(u):
        return jnp.moveaxis(jnp.stack([u[0], jnp.flip(u[1], axis=1)]), 2, 0)

    xs = (to_time(r), to_time(decay), to_time(k), to_time(v), to_time(-kk), to_time(kk * a))
    S0 = jnp.zeros((2, bsz, H, N, N), jnp.float32)
    _, ys = lax.scan(rwkv7_step, S0, xs)
    ys = jnp.moveaxis(ys, 0, 2)
    y = ys[0] + jnp.flip(ys[1], axis=1)
    mean = jnp.mean(y, axis=-1, keepdims=True)
    var = jnp.mean(jnp.square(y - mean), axis=-1, keepdims=True)
    y = (y - mean) * lax.rsqrt(var + LN_X_EPS) * lnx_w.astype(jnp.float32).reshape(H, N) \
        + lnx_b.astype(jnp.float32).reshape(H, N)
    r0 = zr[..., :G].astype(jnp.float32).reshape(bsz, t, H, N)
    k0 = zr[..., G:2 * G].astype(jnp.float32).reshape(bsz, t, H, N)
    v0 = zr[..., 2 * G:3 * G].astype(jnp.float32).reshape(bsz, t, H, N)
    y = y + jnp.sum(r0 * k0 * r_k.astype(jnp.float32), axis=-1, keepdims=True) * v0
    g = (jax.nn.sigmoid(lg) @ g2).astype(jnp.float32)
    return (y.reshape(bsz, t, G) * g).astype(zr.dtype)


def multiscale_pool(u, pool_w, pool_scale):
    bsz, t, _ = u.shape
    ug = u.reshape(bsz, t, POOL_GROUPS, POOL_CH).astype(jnp.float32)
    cs = jnp.pad(jnp.cumsum(ug, axis=1), ((0, 0), (1, 0), (0, 0), (0, 0)))
    pos = jnp.arange(t)[:, None]
    win = jnp.array(POOL_WINDOWS, jnp.int32)[None, :]
    lo = jnp.clip(pos - win // 2, 0, t)
    hi = jnp.clip(pos - win // 2 + win, 0, t)
    grp = jnp.arange(POOL_GROUPS)[None, :]
    mean = (cs[:, hi, grp] - cs[:, lo, grp]) / (hi - lo).astype(jnp.float32)[None, :, :, None]
    d = (mean - ug).astype(u.dtype)
    y = jnp.einsum('btgc,gcd->btgd', d, pool_w) * pool_scale.reshape(POOL_GROUPS, POOL_CH)
    return y.reshape(bsz, t, POOL_GROUPS * POOL_CH)


def short_gated_conv(zc, conv_w):
    b_gate = zc[..., :GROUP_W]
    c_gate = zc[..., GROUP_W:2 * GROUP_W]
    hin = zc[..., 2 * GROUP_W:]
    u = lax.conv_general_dilated(c_gate * hin, conv_w[:, None, :].astype(zc.dtype), window_strides=(1,),
                                 padding=((CONV_K // 2, CONV_K // 2),),
                                 dimension_numbers=('NWC', 'WIO', 'NWC'), feature_group_count=GROUP_W)
    return b_gate * u


def hybrid_mixer(h, w_in, w_out, rpb, mu, w0, w2, a0, a2, k_k, k_a, r_k, lnx_w, lnx_b, g2,
                 pool_w, pool_scale, conv_w):
    G = GROUP_W
    z = h @ w_in
    o = 0
    za = z[..., o:o + 3 * G]; o += 3 * G
    zr = z[..., o:o + RW_SHIFT_W]; o += RW_SHIFT_W
    lg = z[..., o:o + GATE_LORA]; o += GATE_LORA
    zp = z[..., o:o + G]; o += G
    zc = z[..., o:]
    y_a = neighbourhood_attention(za[..., :G], za[..., G:2 * G], za[..., 2 * G:], rpb)
    y_b = rwkv7_bidirectional(zr, lg, mu, w0, w2, a0, a2, k_k, k_a, r_k, lnx_w, lnx_b, g2)
    y_c = multiscale_pool(zp, pool_w, pool_scale)
    y_d = short_gated_conv(zc, conv_w)
    return jnp.concatenate([y_a, y_b, y_c, y_d], axis=-1) @ w_out


def swiglu(h, w1, w3, w2):
    return (jax.nn.silu(h @ w1) * (h @ w3)) @ w2


def moe_swiglu(h, router_w, w1, w3, w2):
    bsz, t, d = h.shape
    n = bsz * t
    xf = h.reshape(n, d)
    logits = (xf @ router_w).astype(jnp.float32)
    top_val, top_idx = lax.top_k(logits, TOP_K)
    gates = jax.nn.softmax(top_val, axis=-1)
    m = n * TOP_K
    flat_e = top_idx.reshape(m)
    flat_tok = jnp.arange(m, dtype=jnp.int32) // TOP_K
    flat_g = gates.reshape(m)
    order = jnp.argsort(flat_e)
    se = flat_e[order]
    counts = jnp.bincount(flat_e, length=N_EXPERTS)
    group_start = jnp.cumsum(counts) - counts
    padded = (counts + MOE_BLOCK - 1) // MOE_BLOCK * MOE_BLOCK
    padded_end = jnp.cumsum(padded)
    padded_start = padded_end - padded
    dest = padded_start[se] + (jnp.arange(m) - group_start[se])
    n_blocks = (m + N_EXPERTS * MOE_BLOCK + MOE_BLOCK - 1) // MOE_BLOCK
    p_rows = n_blocks * MOE_BLOCK
    buf_tok = jnp.zeros((p_rows,), jnp.int32).at[dest].set(flat_tok[order])
    buf_g = jnp.zeros((p_rows,), jnp.float32).at[dest].set(flat_g[order])
    block_e = jnp.minimum(jnp.searchsorted(padded_end, jnp.arange(n_blocks) * MOE_BLOCK, side='right'),
                          N_EXPERTS - 1)
    xb = xf[buf_tok].reshape(n_blocks, MOE_BLOCK, d)

    def expert_block(args):
        xblk, e = args
        return (jax.nn.silu(xblk @ w1[e]) * (xblk @ w3[e])) @ w2[e]

    yb = lax.map(expert_block, (xb, block_e)).reshape(p_rows, d)
    out = jnp.zeros((n, d), h.dtype).at[buf_tok].add(yb * buf_g[:, None].astype(yb.dtype))
    return out.reshape(bsz, t, d)


def setup_inputs(seed: int = 0) -> dict:
    key = jax.random.key(seed)
    keys = iter(jax.random.split(key, 32))

    def nrm(shape, std):
        return std * jax.random.normal(next(keys), shape, jnp.float32)

    def unif(shape, lo, hi):
        return jax.random.uniform(next(keys), shape, jnp.float32, lo, hi)

    G, L = GROUP_W, DEPTH
    return {
        'x': nrm((BATCH, SEQ, D_MODEL), 1.0),
        'c': nrm((BATCH, D_MODEL), 1.0),
        'ada_w': nrm((L, D_MODEL, 6 * D_MODEL), 0.5 * D_MODEL ** -0.5),
        'ada_b': nrm((L, 6 * D_MODEL), 0.02),
        'norm_g': 1.0 + nrm((L, 4, D_MODEL), 0.05),
        'w_in': nrm((L, D_MODEL, IN_W), D_MODEL ** -0.5),
        'w_out': nrm((L, N_GROUPS * G, D_MODEL), (N_GROUPS * G) ** -0.5),
        'na_rpb': nrm((L, NA_HEADS, 2 * WIN_H - 1, 2 * WIN_W - 1), 0.05),
        'rw_mu': unif((L, 2, RW_SHIFT_W), 0.0, 1.0),
        'rw_w0': unif((L, 2, G), -6.0, -1.0),
        'rw_w2': nrm((L, 2, DECAY_LORA, G), 0.5 * DECAY_LORA ** -0.5),
        'rw_a0': nrm((L, 2, G), 0.1),
        'rw_a2': nrm((L, 2, AAA_LORA, G), 0.5 * AAA_LORA ** -0.5),
        'rw_k_k': 0.85 + nrm((L, G), 0.05),
        'rw_k_a': 1.0 + nrm((L, G), 0.05),
        'rw_r_k': nrm((L, RWKV_HEADS, RWKV_HEAD_DIM), 0.1),
        'rw_lnx_w': 1.0 + nrm((L, G), 0.05),
        'rw_lnx_b': nrm((L, G), 0.02),
        'rw_g2': nrm((L, GATE_LORA, G), GATE_LORA ** -0.5),
        'pool_w': nrm((L, POOL_GROUPS, POOL_CH, POOL_CH), POOL_CH ** -0.5),
        'pool_scale': 1.0 + nrm((L, G), 0.1),
        'conv_w': nrm((L, CONV_K, G), CONV_K ** -0.5),
        'ffn_w1': nrm((N_DENSE, D_MODEL, FFN_DIM), D_MODEL ** -0.5),
        'ffn_w3': nrm((N_DENSE, D_MODEL, FFN_DIM), D_MODEL ** -0.5),
        'ffn_w2': nrm((N_DENSE, FFN_DIM, D_MODEL), FFN_DIM ** -0.5),
        'router_w': nrm((N_MOE, D_MODEL, N_EXPERTS), D_MODEL ** -0.5),
        'moe_w1': nrm((N_MOE, N_EXPERTS, D_MODEL, EXPERT_FFN), D_MODEL ** -0.5),
        'moe_w3': nrm((N_MOE, N_EXPERTS, D_MODEL, EXPERT_FFN), D_MODEL ** -0.5),
        'moe_w2': nrm((N_MOE, N_EXPERTS, EXPERT_FFN, D_MODEL), EXPERT_FFN ** -0.5),
    }


def reference(x, c, ada_w, ada_b, norm_g, w_in, w_out, na_rpb, rw_mu, rw_w0, rw_w2, rw_a0, rw_a2,
              rw_k_k, rw_k_a, rw_r_k, rw_lnx_w, rw_lnx_b, rw_g2, pool_w, pool_scale, conv_w,
              ffn_w1, ffn_w3, ffn_w2, router_w, moe_w1, moe_w3, moe_w2):
    for l in range(DEPTH):
        mod = jax.nn.silu(c) @ ada_w[l] + ada_b[l]
        sh_m, sc_m, gt_m, sh_f, sc_f, gt_f = jnp.split(mod[:, None, :], 6, axis=-1)
        h = rms_norm(x, norm_g[l, 0]) * (1.0 + sc_m) + sh_m
        y = hybrid_mixer(h, w_in[l], w_out[l], na_rpb[l], rw_mu[l], rw_w0[l], rw_w2[l], rw_a0[l], rw_a2[l],
                         rw_k_k[l], rw_k_a[l], rw_r_k[l], rw_lnx_w[l], rw_lnx_b[l], rw_g2[l],
                         pool_w[l], pool_scale[l], conv_w[l])
        x = x + gt_m * rms_norm(y, norm_g[l, 1])
        h = rms_norm(x, norm_g[l, 2]) * (1.0 + sc_f) + sh_f
        if l % 2 == 0:
            y = swiglu(h, ffn_w1[l // 2], ffn_w3[l // 2], ffn_w2[l // 2])
        else:
            y = moe_swiglu(h, router_w[l // 2], moe_w1[l // 2], moe_w3[l // 2], moe_w2[l // 2])
        x = x + gt_f * rms_norm(y, norm_g[l, 3])
    return x
```

```python
import os
import numpy as np
from contextlib import ExitStack
import concourse.bass as bass
import concourse.mybir as mybir
from concourse.bass_utils import run_bass_kernel_spmd


ENGS = ('pe', 'act', 'dve', 'pool', 'sp')
DMAQ = ('sp', 'act', 'pool')
NL = 8


class KB:
    def __init__(self, nc):
        self.nc = nc
        self.phase = 0

    def finish(self):
        pass

    def begin(self):
        self.stack = ExitStack()
        self.stack.__enter__()
        nc = self.nc
        self.phase += 1
        self.sem = {}
        for e in ENGS:
            self.sem[e] = nc.alloc_semaphore(name=f"s{self.phase}_{e}")
        for q in DMAQ:
            for l in range(NL):
                self.sem[('d', q, l)] = nc.alloc_semaphore(name=f"d{self.phase}_{q}{l}")
        self.cnt = {k: 0 for k in self.sem}
        self.seen = {e: {} for e in ENGS}
        self.last_w = {}
        self.rd = {}
        self.ops = {e: [] for e in ENGS}
        self.dma_i = {q: 0 for q in DMAQ}
        return self.stack

    def alloc(self, name, shape, dtype):
        return self.stack.enter_context(self.nc.sbuf_tensor(f"{name}_{self.phase}", list(shape), dtype))

    def palloc(self, name, shape, dtype):
        return self.stack.enter_context(self.nc.psum_tensor(f"{name}_{self.phase}", list(shape), dtype))

    def _deps(self, eng, r, w, is_dma=False):
        waits = {}
        seen = self.seen[eng]

        def need(sk, v, raw):
            if sk == eng and not is_dma and eng == 'pe':
                return
            if seen.get(sk, 0) < v and waits.get(sk, 0) < v:
                waits[sk] = v

        for k in r:
            lw = self.last_w.get(k)
            if lw:
                need(lw[0], lw[1], True)
        for k in w:
            lw = self.last_w.get(k)
            if lw:
                need(lw[0], lw[1], False)
            for sk, v in self.rd.get(k, {}).items():
                need(sk, v, False)
        for sk, v in waits.items():
            seen[sk] = v
        return list(waits.items())

    def _record(self, sk, v, r, w):
        for k in r:
            d = self.rd.setdefault(k, {})
            if d.get(sk, 0) < v:
                d[sk] = v
        for k in w:
            self.last_w[k] = (sk, v)
            self.rd[k] = {}

    def op(self, eng, fn, r=(), w=()):
        waits = self._deps(eng, r, w)
        self.cnt[eng] += 1
        v = self.cnt[eng]
        self._record(eng, v, r, w)
        self.ops[eng].append((waits, fn, (eng, 1)))

    def dma(self, q, out, in_, r=(), w=()):
        lane = self.dma_i[q] % NL
        self.dma_i[q] += 1
        sk = ('d', q, lane)
        waits = self._deps(q, r, w, is_dma=True)
        prev = self.cnt[sk]
        if prev > 0 and self.seen[q].get(sk, 0) < prev:
            waits.append((sk, prev))
            self.seen[q][sk] = prev
        v = prev + 16
        self.cnt[sk] = v
        self._record(sk, v, r, w)
        self.ops[q].append((waits, (lambda e, o=out, i=in_: e.dma_start(out=o, in_=i)), (sk, 16)))

    def end(self):
        final = dict(self.cnt)
        sem = self.sem
        ops = self.ops

        def mk(e):
            def body(eo):
                for waits, fn, inc in ops[e]:
                    for sk, v in waits:
                        eo.wait_ge(sem[sk], v)
                    ins = fn(eo)
                    ins.then_inc(sem[inc[0]], inc[1])
                for sk, v in final.items():
                    if v > 0:
                        eo.wait_ge(sem[sk], v)
            return body

        with self.nc.Block() as block:
            block.tensor(mk('pe'))
            block.scalar(mk('act'))
            block.vector(mk('dve'))
            block.gpsimd(mk('pool'))
            block.sync(mk('sp'))
        self.stack.__exit__(None, None, None)
        self.nc.all_engine_barrier()
        self.nc.clear_and_free_semaphores(list(self.sem.values()))
        self.nc.all_engine_barrier()
        self.ops = None


F32 = mybir.dt.float32
BF16 = mybir.dt.bfloat16
AF = mybir.ActivationFunctionType
ALU = mybir.AluOpType
AX = mybir.AxisListType

T = 4096
D = 1024
G = 256
INW = 2688
NTT = 32
RMS_EPS = 1e-6


class Ctx:
    pass


def declare(nc, kinds=None):
    kinds = kinds or {}
    C = Ctx()
    C.nc = nc
    C.kb = KB(nc)

    def din(name, shape, dt=F32):
        return nc.dram_tensor(name, list(shape), dt, kind="ExternalInput").ap()

    def scr(name, shape, dt):
        return nc.dram_tensor(name, list(shape), dt, kind=kinds.get(name, "Internal")).ap()

    C.x = din("x", [T, D])
    C.c_t = din("c_t", [128, 8])
    C.ada_w = din("ada_w", [2, D, 6 * D])
    C.ada_b = din("ada_b", [2, 6 * D])
    C.norm_g = din("norm_g", [2, 4, D])
    C.w_in = din("w_in", [2, D, INW])
    C.w_out = din("w_out", [2, D, D])
    C.ident = din("ident", [128, 128])
    C.out = nc.dram_tensor("out", [T, D], F32, kind="ExternalOutput").ap()
    C.zT_bf = scr("zT_bf", [INW, T], BF16)
    C.zT_rw = scr("zT_rw", [896, T], F32)
    C.vtok = scr("vtok", [T, G], BF16)
    C.yT = scr("yT", [D, T], BF16)
    C.h2T = scr("h2T", [D, T], BF16)
    return C


def alloc_persistent(C, stack):
    nc = C.nc
    C.modt = stack.enter_context(nc.sbuf_tensor("modt", [128, 6, D], F32))
    C.identb = stack.enter_context(nc.sbuf_tensor("identb", [128, 128], BF16))
    C.identf = stack.enter_context(nc.sbuf_tensor("identf", [128, 128], F32))
    C.cst = stack.enter_context(nc.sbuf_tensor("cst", [128, 8], F32))
    C.ones_f = stack.enter_context(nc.sbuf_tensor("ones_f", [128, 128], F32))
    C.ones_b = stack.enter_context(nc.sbuf_tensor("ones_b", [128, 128], BF16))


def phase_init(C):
    kb = C.kb
    kb.begin()
    kb.dma('sp', C.identf[:], C.ident[:, :], w=['identf'])
    kb.dma('pool', C.identb[:], C.ident[:, :], w=['identb'])
    kb.op('dve', lambda e: e.memset(C.cst[:, 0:1], RMS_EPS), w=['cst'])
    kb.op('dve', lambda e: e.memset(C.cst[:, 1:2], 64e-5), w=['cst'])
    kb.op('dve', lambda e: e.memset(C.cst[:, 2:3], 0.0), w=['cst'])
    kb.op('dve', lambda e: e.memset(C.cst[:, 3:4], 1.0), w=['cst'])
    kb.op('dve', lambda e: e.memset(C.ones_f[:], 1.0), w=['ones_f'])
    kb.op('dve', lambda e: e.memset(C.ones_b[:], 1.0), w=['ones_b'])
    kb.end()


def phase_mod(C, l):
    kb = C.kb
    nc = C.nc
    kb.begin()
    ct = kb.alloc("ct", [128, 8], F32)
    sc = kb.alloc("sc", [128, 8], F32)
    scb = kb.alloc("scb", [128, 8, 128], F32)
    adab = kb.alloc("adab", [1, 6 * D], F32)
    gbc = kb.alloc("gbc", [128, 4, D], F32)
    wblk = [kb.alloc(f"wblk{i}", [128, 8, 512], F32) for i in range(2)]
    ps = [kb.palloc(f"mps{i}", [128, 512], F32) for i in range(2)]

    kb.dma('sp', ct[:], C.c_t[:, :], w=['ct'])
    kb.dma('sp', adab[:], C.ada_b[l:l + 1, :], w=['adab'])
    for j in range(4):
        kb.dma('sp', gbc[:, j, :], C.norm_g[l, j, :].partition_broadcast(128), w=[('gbc', j)])
    kb.op('act', lambda e: e.activation(out=sc[:], in_=ct[:], func=AF.Silu), r=['ct'], w=['sc'])
    kb.op('dve', lambda e: e.tensor_copy(out=scb[:], in_=sc[:, 0:8].unsqueeze(2).to_broadcast([128, 8, 128])),
          r=['sc'], w=['scb'])
    wv = C.ada_w[l].rearrange("(k p) n -> p k n", p=128)
    sect = [(1, 'copy', None), (0, 'a', 0), (2, 'g', 1), (4, 'copy', None), (3, 'a', 2), (5, 'g', 3)]
    for nb in range(12):
        b = nb % 2
        kb.dma('sp', wblk[b][:], wv[:, :, nb * 512:(nb + 1) * 512], w=[('wblk', b)])
        for k in range(8):
            kb.op('pe', lambda e, b=b, k=k: e.matmul(ps[b][:], scb[:, k, :], wblk[b][:, k, :], start=(k == 0), stop=False),
                  r=['scb', ('wblk', b)], w=[('mps', b)])
        kb.op('pe', lambda e, b=b, nb=nb: e.matmul(ps[b][:], C.ones_f[0:1, :], adab[0:1, nb * 512:(nb + 1) * 512],
                                                   start=False, stop=True),
              r=['ones_f', 'adab'], w=[('mps', b)])
        s = nb // 2
        cols = slice((nb % 2) * 512, (nb % 2) * 512 + 512)
        slot, mode, gi = sect[s]
        dst = C.modt[:, slot, cols]
        if mode == 'copy':
            kb.op('act', lambda e, dst=dst, b=b: e.activation(out=dst, in_=ps[b][:], func=AF.Copy),
                  r=[('mps', b)], w=[('modt', slot)])
        elif mode == 'a':
            kb.op('dve', lambda e, dst=dst, b=b, gi=gi, cols=cols: e.scalar_tensor_tensor(
                out=dst, in0=ps[b][:], scalar=1.0, in1=gbc[:, gi, cols], op0=ALU.add, op1=ALU.mult),
                r=[('mps', b), ('gbc', gi)], w=[('modt', slot)])
        else:
            kb.op('dve', lambda e, dst=dst, b=b, gi=gi, cols=cols: e.tensor_tensor(
                out=dst, in0=ps[b][:], in1=gbc[:, gi, cols], op=ALU.mult),
                r=[('mps', b), ('gbc', gi)], w=[('modt', slot)])
    kb.end()


def emit_rstd(kb, C, ss, rstd, tag, n):
    kb.op('act', lambda e: e.activation(out=rstd[:, 0:n], in_=ss[:, 0:n], func=AF.Sqrt, bias=C.cst[:, 0:1], scale=1.0 / D),
          r=[(tag, 'ss'), 'cst'], w=[(tag, 'rstd')])
    kb.op('dve', lambda e: e.reciprocal(rstd[:, 0:n], rstd[:, 0:n]), r=[(tag, 'rstd')], w=[(tag, 'rstd')])


def phase_A(C, l, xsrc):
    kb = C.kb
    kb.begin()
    win = kb.alloc("win", [128, 8, INW], BF16)
    xin = [[kb.alloc(f"xin{b}_{j}", [128, D], F32) for j in range(4)] for b in range(2)]
    junk = kb.alloc("junk", [128, D], BF16)
    tmp = [kb.alloc(f"tmp{i}", [128, D], F32) for i in range(2)]
    hb = [kb.alloc(f"hb{i}", [128, D], BF16) for i in range(2)]
    hT = [kb.alloc(f"hT{i}", [128, 8, 512], BF16) for i in range(2)]
    ss = [kb.alloc(f"ss{i}", [128, 4], F32) for i in range(2)]
    rstd = [kb.alloc(f"rstd{i}", [128, 4], F32) for i in range(2)]
    stg_bf = [kb.alloc(f"stgb{i}", [128, 512], BF16) for i in range(3)]
    stg_rw = [kb.alloc(f"stgr{i}", [128, 512], F32) for i in range(2)]
    vst = [kb.alloc(f"vst{i}", [128, G], BF16) for i in range(2)]
    pt = [kb.palloc(f"pt{i}", [128, 8, 128], BF16) for i in range(2)]
    zp = [kb.palloc(f"zp{i}", [128, 512], F32) for i in range(3)]
    vp = [kb.palloc(f"vp{i}", [128, G], F32) for i in range(2)]

    wv = C.w_in[l].rearrange("(k p) n -> p k n", p=128)
    for k in range(8):
        kb.dma('pool', win[:, k, :], wv[:, k, :], w=[('win', k)])
    winkeys = [('win', k) for k in range(8)]
    A1 = C.modt[:, 0, :]
    B1 = C.modt[:, 1, :]
    nev = 0
    ntp = 0
    for g in range(8):
        b = g % 2
        for j in range(4):
            tt = g * 4 + j
            kb.dma('sp', xin[b][j][:], xsrc[tt * 128:(tt + 1) * 128, :], w=[('xin', b, j)])
            kb.op('act', lambda e, b=b, j=j: e.activation(out=junk[:], in_=xin[b][j][:], func=AF.Square,
                                                          accum_out=ss[b][:, j:j + 1]),
                  r=[('xin', b, j)], w=['junk', (('A', b), 'ss')])
        emit_rstd(kb, C, ss[b], rstd[b], ('A', b), 4)
        for j in range(4):
            i2 = j % 2
            kb.op('dve', lambda e, b=b, j=j, i2=i2: e.scalar_tensor_tensor(
                out=tmp[i2][:], in0=xin[b][j][:], scalar=rstd[b][:, j:j + 1], in1=A1, op0=ALU.mult, op1=ALU.mult),
                r=[('xin', b, j), (('A', b), 'rstd'), ('modt', 0)], w=[('tmp', i2)])
            kb.op('pool', lambda e, i2=i2: e.tensor_tensor(out=hb[i2][:], in0=tmp[i2][:], in1=B1, op=ALU.add),
                  r=[('tmp', i2), ('modt', 1)], w=[('hb', i2)])
            p = ntp % 2
            ntp += 1
            for k in range(8):
                kb.op('pe', lambda e, p=p, k=k, i2=i2: e.transpose(pt[p][:, k, :], hb[i2][:, k * 128:(k + 1) * 128], C.identb[:]),
                      r=[('hb', i2), 'identb'], w=[('pt', p)])
            kb.op('act', lambda e, p=p, b=b, j=j: e.activation(out=hT[b][:, :, j * 128:(j + 1) * 128], in_=pt[p][:], func=AF.Copy),
                  r=[('pt', p)], w=[('hT', b, j)])
        hkeys = [('hT', b, j) for j in range(4)]
        for j in range(4):
            tt = g * 4 + j
            q = j % 2
            for k in range(8):
                kb.op('pe', lambda e, q=q, k=k, b=b, j=j: e.matmul(vp[q][:], hT[b][:, k, j * 128:(j + 1) * 128], win[:, k, 512:768],
                                                                   start=(k == 0), stop=(k == 7)),
                      r=[('hT', b, j)] + winkeys, w=[('vp', q)])
            kb.op('dve', lambda e, q=q: e.tensor_copy(out=vst[q][:], in_=vp[q][:]), r=[('vp', q)], w=[('vst', q)])
            kb.dma('sp', C.vtok[tt * 128:(tt + 1) * 128, :], vst[q][:], r=[('vst', q)])
        for fc in range(21):
            z = nev % 3
            for k in range(8):
                kb.op('pe', lambda e, z=z, k=k, b=b, fc=fc: e.matmul(zp[z][:], win[:, k, fc * 128:(fc + 1) * 128], hT[b][:, k, :],
                                                                     start=(k == 0), stop=(k == 7)),
                      r=hkeys + winkeys, w=[('zp', z)])
            is_rw = 6 <= fc < 13
            eng = 'act' if nev % 2 == 0 else 'dve'
            if is_rw:
                s = fc % 2
                if eng == 'act':
                    kb.op('act', lambda e, z=z, s=s: e.activation(out=stg_rw[s][:], in_=zp[z][:], func=AF.Copy),
                          r=[('zp', z)], w=[('stgr', s)])
                else:
                    kb.op('dve', lambda e, z=z, s=s: e.tensor_copy(out=stg_rw[s][:], in_=zp[z][:]),
                          r=[('zp', z)], w=[('stgr', s)])
                kb.dma('sp', C.zT_rw[(fc - 6) * 128:(fc - 5) * 128, g * 512:(g + 1) * 512], stg_rw[s][:], r=[('stgr', s)])
            else:
                s = nev % 3
                if eng == 'act':
                    kb.op('act', lambda e, z=z, s=s: e.activation(out=stg_bf[s][:], in_=zp[z][:], func=AF.Copy),
                          r=[('zp', z)], w=[('stgb', s)])
                else:
                    kb.op('dve', lambda e, z=z, s=s: e.tensor_copy(out=stg_bf[s][:], in_=zp[z][:]),
                          r=[('zp', z)], w=[('stgb', s)])
                kb.dma('sp', C.zT_bf[fc * 128:(fc + 1) * 128, g * 512:(g + 1) * 512], stg_bf[s][:], r=[('stgb', s)])
            nev += 1
    kb.end()


PP = {}
_pp_items = [('pool_scale', 2), ('conv_w', 6), ('mu0', 7), ('mu1', 7), ('w0_0', 2), ('w0_1', 2), ('a0_0', 2), ('a0_1', 2),
             ('k_k', 2), ('k_a', 2), ('lnx_w', 2), ('lnx_b', 2), ('r_k', 2)]
_c = 0
for _n, _k in _pp_items:
    PP[_n] = _c
    _c += _k
NPP = _c


def host_pack_pp(inp):
    pp = np.zeros((2, 128, NPP), np.float32)

    def put(l, name, vec, off=0):
        n = vec.shape[0]
        nch = (n + 127) // 128
        for c in range(nch):
            seg = vec[c * 128:(c + 1) * 128]
            pp[l, :seg.shape[0], PP[name] + off + c] = seg
    for l in range(2):
        put(l, 'pool_scale', inp['pool_scale'][l])
        for k in range(3):
            put(l, 'conv_w', inp['conv_w'][l, k], off=2 * k)
        for e in range(2):
            put(l, f'mu{e}', inp['rw_mu'][l, e])
            put(l, f'w0_{e}', inp['rw_w0'][l, e])
            put(l, f'a0_{e}', inp['rw_a0'][l, e])
        put(l, 'k_k', inp['rw_k_k'][l])
        put(l, 'k_a', inp['rw_k_a'][l])
        put(l, 'lnx_w', inp['rw_lnx_w'][l])
        put(l, 'lnx_b', inp['rw_lnx_b'][l])
        put(l, 'r_k', inp['rw_r_k'][l].reshape(-1))
    return pp


def host_pool_rc():
    wins = (2, 4, 8, 16)
    t = np.arange(T)
    rc = np.zeros((2, 128, T), np.float32)
    for g, w in enumerate(wins):
        lo = np.clip(t - w // 2, 0, T)
        hi = np.clip(t - w // 2 + w, 0, T)
        r = (1.0 / (hi - lo)).astype(np.float32)
        rc[g // 2, (g % 2) * 64:(g % 2) * 64 + 64, :] = r[None, :]
    return rc


def declare2(C, nc):
    def din(name, shape, dt=F32):
        return nc.dram_tensor(name, list(shape), dt, kind="ExternalInput").ap()
    C.pp = din("pp", [2, 128, NPP])
    C.pool_rc = din("pool_rc", [2, 128, T])
    C.pool_w = din("pool_w", [2, 4, 64, 64])


def phase_DE(C, l):
    kb = C.kb
    kb.begin()
    L = T + 32
    ppt = kb.alloc("ppt", [128, NPP], F32)
    kb.dma('sp', ppt[:], C.pp[l], w=['ppt'])
    zt = [kb.alloc(f"zt{i}", [128, T], BF16) for i in range(8)]
    for i in range(8):
        kb.dma('sp', zt[i][:], C.zT_bf[(13 + i) * 128:(14 + i) * 128, :], w=[('zt', i)])
    rc = [kb.alloc(f"rc{i}", [128, T], F32) for i in range(2)]
    for i in range(2):
        kb.dma('sp', rc[i][:], C.pool_rc[i], w=[('rc', i)])
    pwb = [kb.alloc(f"pwb{i}", [128, 128], BF16) for i in range(2)]
    for ci in range(2):
        kb.op('dve', lambda e, ci=ci: e.memset(pwb[ci][:], 0.0), w=[('pwb', ci)])
        for h in range(2):
            kb.dma('pool', pwb[ci][h * 64:(h + 1) * 64, h * 64:(h + 1) * 64], C.pool_w[l, ci * 2 + h], r=[], w=[('pwb', ci)])
    upad = kb.alloc("upad", [128, L], F32)
    bufA = kb.alloc("bufA", [128, L], F32)
    bufB = kb.alloc("bufB", [128, L], F32)
    dT = kb.alloc("dT", [128, T], BF16)
    yst = [kb.alloc(f"yst{i}", [128, T], BF16) for i in range(2)]
    pp_ = [kb.palloc(f"pps{i}", [128, 512], F32) for i in range(2)]
    kb.op('dve', lambda e: e.memset(upad[:, 0:16], 0.0), w=['upad'])
    kb.op('dve', lambda e: e.memset(upad[:, 16 + T:L], 0.0), w=['upad'])
    for ci in range(2):
        kb.op('act', lambda e, ci=ci: e.activation(out=upad[:, 16:16 + T], in_=zt[ci][:], func=AF.Copy),
              r=[('zt', ci)], w=['upad'])
        kb.op('dve', lambda e: e.tensor_tensor(out=bufA[:, 1:L], in0=upad[:, 0:L - 1], in1=upad[:, 1:L], op=ALU.add),
              r=['upad'], w=['bufA'])
        kb.op('dve', lambda e: e.tensor_tensor(out=bufB[:, 2:L - 1], in0=bufA[:, 1:L - 2], in1=bufA[:, 3:L], op=ALU.add),
              r=['bufA'], w=['bufB'])
        if ci == 1:
            kb.op('dve', lambda e: e.tensor_tensor(out=bufA[:, 4:L - 3], in0=bufB[:, 2:L - 5], in1=bufB[:, 6:L - 1], op=ALU.add),
                  r=['bufB'], w=['bufA'])
            kb.op('dve', lambda e: e.tensor_tensor(out=bufB[:, 8:L - 7], in0=bufA[:, 4:L - 11], in1=bufA[:, 12:L - 3], op=ALU.add),
                  r=['bufA'], w=['bufB'])
        kb.op('pool', lambda e, ci=ci: e.tensor_tensor(out=bufA[0:64, 16:16 + T], in0=bufA[0:64, 16:16 + T], in1=rc[ci][0:64, :], op=ALU.mult),
              r=['bufA', ('rc', ci)], w=['bufA'])
        kb.op('dve', lambda e, ci=ci: e.tensor_tensor(out=bufB[64:128, 16:16 + T], in0=bufB[64:128, 16:16 + T], in1=rc[ci][64:128, :], op=ALU.mult),
              r=['bufB', ('rc', ci)], w=['bufB'])
        kb.op('pool', lambda e: e.tensor_tensor(out=dT[0:64, :], in0=bufA[0:64, 16:16 + T], in1=upad[0:64, 16:16 + T], op=ALU.subtract),
              r=['bufA', 'upad'], w=['dT'])
        kb.op('dve', lambda e: e.tensor_tensor(out=dT[64:128, :], in0=bufB[64:128, 16:16 + T], in1=upad[64:128, 16:16 + T], op=ALU.subtract),
              r=['bufB', 'upad'], w=['dT'])
        for nt in range(8):
            q = nt % 2
            kb.op('pe', lambda e, q=q, nt=nt, ci=ci: e.matmul(pp_[q][:], pwb[ci][:], dT[:, nt * 512:(nt + 1) * 512], start=True, stop=True),
                  r=[('pwb', ci), 'dT'], w=[('pps', q)])
            col = PP['pool_scale'] + ci
            kb.op('act', lambda e, q=q, nt=nt, ci=ci, col=col: e.activation(
                out=yst[ci][:, nt * 512:(nt + 1) * 512], in_=pp_[q][:], func=AF.Copy, scale=ppt[:, col:col + 1]),
                r=[('pps', q), 'ppt'], w=[('yst', ci)])
        kb.dma('sp', C.yT[512 + ci * 128:512 + (ci + 1) * 128, :], yst[ci][:], r=[('yst', ci)])
    up2 = upad
    acc = bufA
    yc = yst
    kb.op('dve', lambda e: e.memset(up2[:, 0:1], 0.0), w=['upad'])
    kb.op('dve', lambda e: e.memset(up2[:, T + 1:T + 2], 0.0), w=['upad'])
    for ci in range(2):
        bg, cg, hin = zt[2 + ci], zt[4 + ci], zt[6 + ci]
        kb.op('dve', lambda e, cg=cg, hin=hin: e.tensor_tensor(out=up2[:, 1:T + 1], in0=cg[:], in1=hin[:], op=ALU.mult),
              r=[('zt', 4 + ci), ('zt', 6 + ci)], w=['upad'])
        cw = PP['conv_w']
        kb.op('pool', lambda e, ci=ci, cw=cw: e.tensor_scalar(acc[:, 0:T], up2[:, 1:T + 1], ppt[:, cw + 2 + ci:cw + 3 + ci], None, ALU.mult),
              r=['upad', 'ppt'], w=['bufA'])
        kb.op('dve', lambda e, ci=ci, cw=cw: e.scalar_tensor_tensor(out=acc[:, 0:T], in0=up2[:, 0:T], scalar=ppt[:, cw + ci:cw + ci + 1],
                                                                    in1=acc[:, 0:T], op0=ALU.mult, op1=ALU.add),
              r=['upad', 'ppt', 'bufA'], w=['bufA'])
        kb.op('dve', lambda e, ci=ci, cw=cw: e.scalar_tensor_tensor(out=acc[:, 0:T], in0=up2[:, 2:T + 2], scalar=ppt[:, cw + 4 + ci:cw + 5 + ci],
                                                                    in1=acc[:, 0:T], op0=ALU.mult, op1=ALU.add),
              r=['upad', 'ppt', 'bufA'], w=['bufA'])
        kb.op('pool', lambda e, ci=ci, bg=bg: e.tensor_tensor(out=yc[ci][:], in0=acc[:, 0:T], in1=bg[:], op=ALU.mult),
              r=['bufA', ('zt', 2 + ci)], w=[('yst', ci)])
        kb.dma('sp', C.yT[768 + ci * 128:768 + (ci + 1) * 128, :], yc[ci][:], r=[('yst', ci)])
    kb.end()


def declare3(C, nc, kinds=None):
    kinds = kinds or {}
    C.router_wT = nc.dram_tensor("router_wT", [8, D], F32, kind="ExternalInput").ap()
    C.xmid = nc.dram_tensor("xmid", [T, D], F32, kind=kinds.get("xmid", "Internal")).ap()


def alloc_persistent2(C, stack):
    nc = C.nc
    C.gates = stack.enter_context(nc.sbuf_tensor("gates", [128, NTT, 8], F32))


def phase_F(C, l, xsrc, moe):
    kb = C.kb
    kb.begin()
    wout = kb.alloc("wout", [128, 8, D], BF16)
    wv = C.w_out[l].rearrange("(k p) n -> p k n", p=128)
    for k in range(8):
        kb.dma('pool', wout[:, k, :], wv[:, k, :], w=[('wout', k)])
    wkeys = [('wout', k) for k in range(8)]
    yTg = [kb.alloc(f"yTg{i}", [128, 8, 512], BF16) for i in range(2)]
    xin = [kb.alloc(f"fx{i}", [128, D], F32) for i in range(3)]
    tmp = [kb.alloc(f"ft{i}", [128, D], F32) for i in range(2)]
    xn = [kb.alloc(f"fxn{i}", [128, D], F32) for i in range(2)]
    h2 = [kb.alloc(f"fh{i}", [128, D], F32) for i in range(2)]
    h2b = [kb.alloc(f"fhb{i}", [128, D], BF16) for i in range(2)]
    junk = kb.alloc("fjunk", [128, D], BF16)
    ss = [kb.alloc(f"fss{i}", [128, 4], F32) for i in range(4)]
    rs = [kb.alloc(f"frs{i}", [128, 4], F32) for i in range(4)]
    h2Tg = [kb.alloc(f"h2Tg{i}", [128, 8, 512], BF16) for i in range(2)]
    yps = [kb.palloc(f"yps{i}", [128, D], F32) for i in range(3)]
    pt = [kb.palloc(f"fpt{i}", [128, 8, 128], BF16) for i in range(2)]
    G1 = C.modt[:, 2, :]
    A2 = C.modt[:, 3, :]
    B2 = C.modt[:, 4, :]
    if moe:
        rwb = kb.alloc("rwb", [128, 8, D], F32)
        for e_ in range(8):
            kb.dma('sp', rwb[:, e_, :], C.router_wT[e_, :].partition_broadcast(128), w=[('rwb', e_)])
        lg = [kb.alloc(f"lg{i}", [128, 8], F32) for i in range(2)]
        sm = [kb.alloc(f"sm{i}", [128, 16], F32) for i in range(2)]
        eq1 = [kb.alloc(f"eq1{i}", [128, 8], F32) for i in range(2)]
        eq2 = [kb.alloc(f"eq2{i}", [128, 8], F32) for i in range(2)]
        l2 = [kb.alloc(f"l2{i}", [128, 8], F32) for i in range(2)]
        rjunk = kb.alloc("rjunk", [128, D], BF16)
    xvd = C.yT.rearrange("(k p) t -> p k t", p=128)
    h2v = C.h2T.rearrange("(k p) t -> p k t", p=128)
    NB3 = 3

    def emit_load(g):
        b = g % 2
        kb.dma('sp', yTg[b][:], xvd[:, :, g * 512:(g + 1) * 512], w=[('yTg', b)])

    def emit_mm(tt):
        g, j = tt // 4, tt % 4
        b = g % 2
        i3 = tt % NB3
        if j == 0:
            emit_load(g)
        kb.dma('sp', xin[i3][:], xsrc[tt * 128:(tt + 1) * 128, :], w=[('fx', i3)])
        for nh in range(2):
            for k in range(8):
                kb.op('pe', lambda e, nh=nh, k=k: e.matmul(
                    yps[i3][:, nh * 512:(nh + 1) * 512], yTg[b][:, k, j * 128:(j + 1) * 128], wout[:, k, nh * 512:(nh + 1) * 512],
                    start=(k == 0), stop=(k == 7)),
                    r=[('yTg', b)] + wkeys, w=[('yps', i3)])

    def emit_mid(tt):
        i2 = tt % 2
        i3 = tt % NB3
        i4 = tt % 4
        for nh in range(2):
            kb.op('act', lambda e, nh=nh: e.activation(
                out=junk[:, nh * 512:(nh + 1) * 512], in_=yps[i3][:, nh * 512:(nh + 1) * 512], func=AF.Square,
                accum_out=ss[i4][:, nh:nh + 1]),
                r=[('yps', i3)], w=['fjunk', ('fss', i4)])
        kb.op('dve', lambda e: e.tensor_tensor(out=ss[i4][:, 2:3], in0=ss[i4][:, 0:1], in1=ss[i4][:, 1:2], op=ALU.add),
              r=[('fss', i4)], w=[('fss', i4)])
        kb.op('act', lambda e: e.activation(out=rs[i4][:, 0:1], in_=ss[i4][:, 2:3], func=AF.Sqrt, bias=C.cst[:, 0:1], scale=1.0 / D),
              r=[('fss', i4), 'cst'], w=[('frs', i4)])
        kb.op('dve', lambda e: e.reciprocal(rs[i4][:, 0:1], rs[i4][:, 0:1]), r=[('frs', i4)], w=[('frs', i4)])
        kb.op('dve', lambda e: e.scalar_tensor_tensor(
            out=tmp[i2][:], in0=yps[i3][:], scalar=rs[i4][:, 0:1], in1=G1, op0=ALU.mult, op1=ALU.mult),
            r=[('yps', i3), ('frs', i4), ('modt', 2)], w=[('ft', i2)])
        kb.op('pool', lambda e: e.tensor_tensor(out=xn[i2][:], in0=tmp[i2][:], in1=xin[i3][:], op=ALU.add),
              r=[('ft', i2), ('fx', i3)], w=[('fxn', i2)])
        kb.dma('sp', C.xmid[tt * 128:(tt + 1) * 128, :], xn[i2][:], r=[('fxn', i2)])
        kb.op('act', lambda e: e.activation(out=junk[:], in_=xn[i2][:], func=AF.Square, accum_out=ss[i4][:, 3:4]),
              r=[('fxn', i2)], w=['fjunk', ('fss2', i4)])
        kb.op('act', lambda e: e.activation(out=rs[i4][:, 1:2], in_=ss[i4][:, 3:4], func=AF.Sqrt, bias=C.cst[:, 0:1], scale=1.0 / D),
              r=[('fss2', i4), 'cst'], w=[('frs2', i4)])
        kb.op('dve', lambda e: e.reciprocal(rs[i4][:, 1:2], rs[i4][:, 1:2]), r=[('frs2', i4)], w=[('frs2', i4)])
        kb.op('dve', lambda e: e.scalar_tensor_tensor(
            out=tmp[i2][:], in0=xn[i2][:], scalar=rs[i4][:, 1:2], in1=A2, op0=ALU.mult, op1=ALU.mult),
            r=[('fxn', i2), ('frs2', i4), ('modt', 3)], w=[('ft', i2)])
        kb.op('pool', lambda e: e.tensor_tensor(out=h2[i2][:], in0=tmp[i2][:], in1=B2, op=ALU.add),
              r=[('ft', i2), ('modt', 4)], w=[('fh', i2)])
        kb.op('act', lambda e: e.activation(out=h2b[i2][:], in_=h2[i2][:], func=AF.Copy),
              r=[('fh', i2)], w=[('fhb', i2)])

    def emit_tr(tt):
        g, j = tt // 4, tt % 4
        b = g % 2
        i2 = tt % 2
        for k in range(8):
            kb.op('pe', lambda e, k=k: e.transpose(pt[i2][:, k, :], h2b[i2][:, k * 128:(k + 1) * 128], C.identb[:]),
                  r=[('fhb', i2), 'identb'], w=[('fpt', i2)])
        kb.op('act', lambda e: e.activation(out=h2Tg[b][:, :, j * 128:(j + 1) * 128], in_=pt[i2][:], func=AF.Copy),
              r=[('fpt', i2)], w=[('h2Tg', b)])
        if moe:
            for e_ in range(8):
                kb.op('dve', lambda e, e_=e_: e.scalar_tensor_tensor(
                    out=rjunk[:], in0=h2[i2][:], scalar=1.0, in1=rwb[:, e_, :], op0=ALU.mult, op1=ALU.mult,
                    accum_out=lg[i2][:, e_:e_ + 1]),
                    r=[('fh', i2), ('rwb', e_)], w=['rjunk', ('lg', i2)])
            s = sm[i2]
            kb.op('dve', lambda e: e.reduce_max(out=s[:, 0:1], in_=lg[i2][:], axis=AX.X), r=[('lg', i2)], w=[('sm', i2)])
            kb.op('dve', lambda e: e.tensor_scalar(eq1[i2][:], lg[i2][:], s[:, 0:1], None, ALU.is_equal),
                  r=[('lg', i2), ('sm', i2)], w=[('eq1', i2)])
            kb.op('dve', lambda e: e.scalar_tensor_tensor(out=l2[i2][:], in0=eq1[i2][:], scalar=-1e30, in1=lg[i2][:],
                                                          op0=ALU.mult, op1=ALU.add),
                  r=[('eq1', i2), ('lg', i2)], w=[('l2', i2)])
            kb.op('dve', lambda e: e.reduce_max(out=s[:, 1:2], in_=l2[i2][:], axis=AX.X), r=[('l2', i2)], w=[('sm', i2)])
            kb.op('dve', lambda e: e.tensor_scalar(eq2[i2][:], l2[i2][:], s[:, 1:2], None, ALU.is_equal),
                  r=[('l2', i2), ('sm', i2)], w=[('eq2', i2)])
            kb.op('dve', lambda e: e.tensor_tensor(out=s[:, 2:3], in0=s[:, 1:2], in1=s[:, 0:1], op=ALU.subtract),
                  r=[('sm', i2)], w=[('sm', i2)])
            kb.op('act', lambda e: e.activation(out=s[:, 3:4], in_=s[:, 2:3], func=AF.Exp), r=[('sm', i2)], w=[('sm', i2)])
            kb.op('dve', lambda e: e.tensor_scalar(s[:, 4:5], s[:, 3:4], 1.0, None, ALU.add), r=[('sm', i2)], w=[('sm', i2)])
            kb.op('dve', lambda e: e.reciprocal(s[:, 5:6], s[:, 4:5]), r=[('sm', i2)], w=[('sm', i2)])
            kb.op('dve', lambda e: e.tensor_tensor(out=s[:, 6:7], in0=s[:, 3:4], in1=s[:, 5:6], op=ALU.mult),
                  r=[('sm', i2)], w=[('sm', i2)])
            kb.op('dve', lambda e: e.tensor_scalar(eq1[i2][:], eq1[i2][:], s[:, 5:6], None, ALU.mult),
                  r=[('eq1', i2), ('sm', i2)], w=[('eq1', i2)])
            kb.op('dve', lambda e: e.scalar_tensor_tensor(
                out=C.gates[:, tt, :], in0=eq2[i2][:], scalar=s[:, 6:7], in1=eq1[i2][:], op0=ALU.mult, op1=ALU.add),
                r=[('eq2', i2), ('sm', i2), ('eq1', i2)], w=[('gates', tt)])
        if j == 3:
            kb.dma('sp', h2v[:, :, g * 512:(g + 1) * 512], h2Tg[b][:], r=[('h2Tg', b)])

    LA = 2
    for tt in range(min(LA, NTT)):
        emit_mm(tt)
    for tt in range(NTT):
        emit_mid(tt)
        if tt + LA < NTT:
            emit_mm(tt + LA)
        emit_tr(tt)
    kb.end()


def declare4(C, nc):
    def din(name, shape, dt=F32):
        return nc.dram_tensor(name, list(shape), dt, kind="ExternalInput").ap()
    FF = 3584
    C.ffn_w1 = din("ffn_w1", [D, FF])
    C.ffn_w3 = din("ffn_w3", [D, FF])
    C.ffn_w2 = din("ffn_w2", [FF, D])
    C.moe_w1 = din("moe_w1", [8, D, FF])
    C.moe_w3 = din("moe_w3", [8, D, FF])
    C.moe_w2 = din("moe_w2", [8, FF, D])


def phase_FFN(C, l, moe, tbs=range(4)):
    kb = C.kb
    G2 = C.modt[:, 5, :]
    h2v = C.h2T.rearrange("(k p) t -> p k t", p=128)
    for tb in tbs:
        kb.begin()
        h2t = kb.alloc("h2t", [128, 8, 1024], BF16)
        acc = kb.alloc("acc", [128, 8, D], F32)
        aT = kb.alloc("aT", [128, 14, 1024], BF16)
        w2h = [kb.alloc(f"w2h{i}", [128, 14, D], BF16) for i in range(2)]
        w1b = [kb.alloc(f"w1b{i}", [128, 8, 256], BF16) for i in range(2)]
        w3b = [kb.alloc(f"w3b{i}", [128, 8, 256], BF16) for i in range(2)]
        sg = [kb.alloc(f"sg{i}", [128, 512], BF16) for i in range(2)]
        gp = [kb.palloc(f"gp{i}", [128, 512], F32) for i in range(2)]
        up = [kb.palloc(f"up{i}", [128, 512], F32) for i in range(2)]
        yp = [kb.palloc(f"yp{i}", [128, 512], F32) for i in range(3)]
        for k in range(8):
            kb.dma('sp', h2t[:, k, :], h2v[:, k, tb * 1024:(tb + 1) * 1024], w=[('h2t', k)])
        hkeys = [('h2t', k) for k in range(8)]
        experts = list(range(8)) if moe else [None]
        nblk = 0
        nhalf = 0
        ngu = 0
        nyp = 0
        first = True
        for e_ in experts:
            if moe:
                W1, W3, W2 = C.moe_w1[e_], C.moe_w3[e_], C.moe_w2[e_]
            else:
                W1, W3, W2 = C.ffn_w1, C.ffn_w3, C.ffn_w2
            W1v = W1.rearrange("(k p) n -> p k n", p=128)
            W3v = W3.rearrange("(k p) n -> p k n", p=128)
            W2v = W2.rearrange("(c p) n -> p c n", p=128)
            for fh in range(2):
                hb_ = nhalf % 2
                nhalf += 1
                for blk in range(7):
                    wb = nblk % 2
                    nblk += 1
                    c0 = fh * 1792 + blk * 256
                    kb.dma('pool', w1b[wb][:], W1v[:, :, c0:c0 + 256], w=[('w1b', wb)])
                    kb.dma('pool', w3b[wb][:], W3v[:, :, c0:c0 + 256], w=[('w3b', wb)])
                    if blk == 0:
                        kb.dma('pool', w2h[hb_][:, 0:7, :], W2v[:, fh * 14:fh * 14 + 7, :], w=[('w2h', hb_, 0)])
                    if blk == 1:
                        kb.dma('pool', w2h[hb_][:, 7:14, :], W2v[:, fh * 14 + 7:fh * 14 + 14, :], w=[('w2h', hb_, 1)])
                    for fcl in range(2):
                        fc = blk * 2 + fcl
                        for th in range(2):
                            q = ngu % 2
                            ngu += 1
                            for k in range(8):
                                kb.op('pe', lambda e, q=q, wb=wb, k=k, fcl=fcl, th=th: e.matmul(
                                    gp[q][:], w1b[wb][:, k, fcl * 128:(fcl + 1) * 128], h2t[:, k, th * 512:(th + 1) * 512],
                                    start=(k == 0), stop=(k == 7)), r=[('w1b', wb)] + hkeys, w=[('gp', q)])
                            for k in range(8):
                                kb.op('pe', lambda e, q=q, wb=wb, k=k, fcl=fcl, th=th: e.matmul(
                                    up[q][:], w3b[wb][:, k, fcl * 128:(fcl + 1) * 128], h2t[:, k, th * 512:(th + 1) * 512],
                                    start=(k == 0), stop=(k == 7)), r=[('w3b', wb)] + hkeys, w=[('up', q)])
                            kb.op('act', lambda e, q=q: e.activation(out=sg[q][:], in_=gp[q][:], func=AF.Silu),
                                  r=[('gp', q)], w=[('sg', q)])
                            kb.op('dve', lambda e, q=q, fc=fc, th=th: e.tensor_tensor(
                                out=aT[:, fc, th * 512:(th + 1) * 512], in0=sg[q][:], in1=up[q][:], op=ALU.mult),
                                r=[('sg', q), ('up', q)], w=[('aT', fc, th)])
                akeys = [('aT', fc, th) for fc in range(14) for th in range(2)]
                for j in range(8):
                    tt = tb * 8 + j
                    for nh in range(2):
                        y = nyp % 3
                        nyp += 1
                        for fc in range(14):
                            kb.op('pe', lambda e, y=y, fc=fc, j=j, nh=nh, hb_=hb_: e.matmul(
                                yp[y][:], aT[:, fc, j * 128:(j + 1) * 128], w2h[hb_][:, fc, nh * 512:(nh + 1) * 512],
                                start=(fc == 0), stop=(fc == 13)),
                                r=[('aT', fc, j // 4), ('w2h', hb_, fc // 7)], w=[('yp', y)])
                        dst = acc[:, j, nh * 512:(nh + 1) * 512]
                        akey = ('acc', j, nh)
                        if first:
                            if moe:
                                kb.op('dve', lambda e, dst=dst, y=y, tt=tt, e_=e_: e.tensor_scalar(
                                    dst, yp[y][:], C.gates[:, tt, e_:e_ + 1], None, ALU.mult),
                                    r=[('yp', y), ('gates', tt)], w=[akey])
                            else:
                                kb.op('act', lambda e, dst=dst, y=y: e.activation(out=dst, in_=yp[y][:], func=AF.Copy),
                                      r=[('yp', y)], w=[akey])
                        else:
                            if moe:
                                kb.op('dve', lambda e, dst=dst, y=y, tt=tt, e_=e_: e.scalar_tensor_tensor(
                                    out=dst, in0=yp[y][:], scalar=C.gates[:, tt, e_:e_ + 1], in1=dst, op0=ALU.mult, op1=ALU.add),
                                    r=[('yp', y), ('gates', tt), akey], w=[akey])
                            else:
                                kb.op('dve', lambda e, dst=dst, y=y: e.tensor_tensor(out=dst, in0=yp[y][:], in1=dst, op=ALU.add),
                                      r=[('yp', y), akey], w=[akey])
                first = False
        xm = [kb.alloc(f"xm{i}", [128, D], F32) for i in range(2)]
        t2 = [kb.alloc(f"t2{i}", [128, D], F32) for i in range(2)]
        junk = kb.alloc("gjunk", [128, D], BF16)
        ss = kb.alloc("gss", [128, 8], F32)
        rs = kb.alloc("grs", [128, 8], F32)
        for j in range(8):
            tt = tb * 8 + j
            i2 = j % 2
            kb.dma('sp', xm[i2][:], C.xmid[tt * 128:(tt + 1) * 128, :], w=[('xm', i2)])
            kb.op('act', lambda e, j=j: e.activation(out=junk[:], in_=acc[:, j, :], func=AF.Square, accum_out=ss[:, j:j + 1]),
                  r=[('acc', j, 0), ('acc', j, 1)], w=['gjunk', ('gss', j)])
            kb.op('act', lambda e, j=j: e.activation(out=rs[:, j:j + 1], in_=ss[:, j:j + 1], func=AF.Sqrt, bias=C.cst[:, 0:1], scale=1.0 / D),
                  r=[('gss', j), 'cst'], w=[('grs', j)])
            kb.op('dve', lambda e, j=j: e.reciprocal(rs[:, j:j + 1], rs[:, j:j + 1]), r=[('grs', j)], w=[('grs', j)])
            kb.op('dve', lambda e, j=j, i2=i2: e.scalar_tensor_tensor(
                out=t2[i2][:], in0=acc[:, j, :], scalar=rs[:, j:j + 1], in1=G2, op0=ALU.mult, op1=ALU.mult),
                r=[('acc', j, 0), ('acc', j, 1), ('grs', j), ('modt', 5)], w=[('t2', i2)])
            kb.op('pool', lambda e, i2=i2: e.tensor_tensor(out=t2[i2][:], in0=t2[i2][:], in1=xm[i2][:], op=ALU.add),
                  r=[('t2', i2), ('xm', i2)], w=[('t2', i2)])
            kb.dma('sp', C.out[tt * 128:(tt + 1) * 128, :], t2[i2][:], r=[('t2', i2)])
        kb.end()


def host_na_table(rpb):
    tab = np.full((14, 4, 128, 256), -30000.0, np.float32)
    qc = np.arange(64)
    cs = np.clip(qc - 8, 0, 48)
    kinds = [('g', m) for m in range(6)] + [('e0', m) for m in range(4)] + [('e15', m) for m in range(4)]
    for ti, (kind, m) in enumerate(kinds):
        for a in range(2):
            for i in range(4):
                dr = (2 * m + a - i) if kind == 'e0' else (2 * m - 4 + a - i)
                if kind == 'g' and not (-4 <= dr <= 3):
                    continue
                for kc in range(64):
                    v = (kc >= cs) & (kc < cs + 16)
                    tab[ti, :, a * 64 + kc, i * 64 + qc[v]] = rpb[:, dr + 7, kc - qc[v] + 15].T
    return np.ascontiguousarray(tab.transpose(2, 0, 1, 3))


def declare5(C, nc):
    C.na_tab = nc.dram_tensor("na_tab", [2, 128, 14, 4, 256], F32, kind="ExternalInput").ap()


def phase_NA(C, l):
    import os
    STG = int(os.environ.get("NA_STAGE", "9"))
    kb = C.kb
    kb.begin()
    q2 = [kb.alloc(f"q2{i}", [128, T], BF16) for i in range(2)]
    k2 = [kb.alloc(f"k2{i}", [128, T], BF16) for i in range(2)]
    vt = kb.alloc("vt", [128, NTT, G], BF16)
    btab = kb.alloc("btab", [128, 14, 4, 256], BF16)
    yall = kb.alloc("yall", [64, 4, T], BF16)
    sb = [kb.alloc(f"sb{i}", [128, 2, 256], F32) for i in range(4)]
    pT = [kb.alloc(f"pT{i}", [128, 2, 256], BF16) for i in range(4)]
    rden = [kb.alloc(f"rden{i}", [64, 2, 256], F32) for i in range(2)]
    sp = [kb.palloc(f"sp{i}", [128, 2, 256], F32) for i in range(4)]
    ops_ = [kb.palloc(f"ops{i}", [64, 512], F32) for i in range(2)]
    dps = [kb.palloc(f"dps{i}", [64, 512], F32) for i in range(2)]
    for hp in range(2):
        kb.dma('sp', q2[hp][:], C.zT_bf[hp * 128:(hp + 1) * 128, :], w=[('q2', hp)])
        kb.dma('sp', k2[hp][:], C.zT_bf[256 + hp * 128:256 + (hp + 1) * 128, :], w=[('k2', hp)])
    kb.dma('sp', vt[:], C.vtok.rearrange("(n p) c -> p n c", p=128), w=['vt'])
    for ti in range(14):
        kb.dma('pool', btab[:, ti], C.na_tab[l, :, ti], w=['btab'])
    kz = [[kb.alloc(f"kz{hp}{h2}", [128, T], BF16) for h2 in range(2)] for hp in range(2)]
    for hp in range(2):
        for h2 in range(2):
            oth = slice((1 - h2) * 64, (2 - h2) * 64)
            me = slice(h2 * 64, (h2 + 1) * 64)
            kb.op('pool', lambda e, hp=hp, h2=h2, oth=oth: e.memset(kz[hp][h2][oth, :], 0.0), w=[('kz', hp, h2)])
            kb.op('dve', lambda e, hp=hp, h2=h2, me=me: e.tensor_copy(out=kz[hp][h2][me, :], in_=k2[hp][me, :]),
                  r=[('k2', hp)], w=[('kz', hp, h2)])
    items = []
    for qg in range(16):
        if qg == 0:
            tiles = [(m, 6 + m) for m in range(4)]
        elif qg == 15:
            tiles = [(28 + m, 10 + m) for m in range(4)]
        else:
            tiles = [(2 * qg - 2 + m, m) for m in range(6)]
        for hp in range(2):
            for idx, (kt, ti) in enumerate(tiles):
                items.append((qg, hp, idx, kt, ti, len(tiles)))

    def emit_score(j):
        qg, hp, idx, kt, ti, nt = items[j]
        s = j % 4
        qs = slice(qg * 256, (qg + 1) * 256)
        for h2 in range(2):
            kb.op('pe', lambda e, h2=h2: e.matmul(sp[s][:, h2, :], kz[hp][h2][:, kt * 128:(kt + 1) * 128], q2[hp][:, qs], start=True, stop=True),
                  r=[('kz', hp, h2), ('q2', hp)], w=[('sp', s)])

    def emit_post(j):
        qg, hp, idx, kt, ti, nt = items[j]
        s = j % 4
        kb.op('dve', lambda e: e.scalar_tensor_tensor(
            out=sb[s][:], in0=sp[s][:], scalar=0.125, in1=btab[:, ti, 2 * hp:2 * hp + 2, :], op0=ALU.mult, op1=ALU.add),
            r=[('sp', s), 'btab'], w=[('sb', s)])
        kb.op('act', lambda e: e.activation(out=pT[s][:], in_=sb[s][:], func=AF.Exp), r=[('sb', s)], w=[('pT', s)])

    def emit_pv(j):
        qg, hp, idx, kt, ti, nt = items[j]
        s = j % 4
        qs = slice(qg * 256, (qg + 1) * 256)
        for h2 in range(2):
            h = 2 * hp + h2
            kb.op('pe', lambda e, h2=h2, h=h: e.matmul(
                ops_[h2][:, 0:256], vt[:, kt, h * 64:(h + 1) * 64], pT[s][:, h2, :], start=(idx == 0), stop=(idx == nt - 1)),
                r=['vt', ('pT', s)], w=[('ops', h2)])
            kb.op('pe', lambda e, h2=h2: e.matmul(
                dps[h2][:, 0:256], C.ones_b[:, 0:64], pT[s][:, h2, :], start=(idx == 0), stop=(idx == nt - 1)),
                r=['ones_b', ('pT', s)], w=[('dps', h2)])
        if idx == nt - 1:
            for h2 in range(2):
                kb.op('dve', lambda e, h2=h2: e.reciprocal(rden[0][:, h2, :], dps[h2][:, 0:256]), r=[('dps', h2)], w=[('rden', h2)])
                kb.op('dve', lambda e, h2=h2: e.tensor_tensor(
                    out=yall[:, 2 * hp + h2, qs], in0=ops_[h2][:, 0:256], in1=rden[0][:, h2, :], op=ALU.mult),
                    r=[('ops', h2), ('rden', h2)], w=[('yall', hp)])

    NI = len(items)
    LA = 2
    for j in range(min(LA, NI)):
        emit_score(j)
    for j in range(NI):
        emit_post(j)
        if j + LA < NI:
            emit_score(j + LA)
        emit_pv(j)
    for h in range(4):
        kb.dma('sp', C.yT[h * 64:(h + 1) * 64, :], yall[:, h, :], r=[('yall', h // 2)])
    kb.end()


TBK = 1024
NCH = 32
E05 = 0.6065306597126334


def host_rw_masks():
    idx = np.arange(128)
    s = idx[:, None]
    t = idx[None, :]
    m = np.zeros((2, 128, 256), np.float32)
    m[0, :, 0:128] = (t > s)
    m[0, :, 128:256] = (t >= s)
    m[1, :, 0:128] = (t < s)
    m[1, :, 128:256] = (t <= s)
    return m


def host_blk():
    b = np.zeros((128, 128), np.float32)
    b[0:64, 0:64] = 1.0
    b[64:128, 64:128] = 1.0
    return b


def declare6(C, nc, kinds=None):
    kinds = kinds or {}

    def din(name, shape, dt=F32):
        return nc.dram_tensor(name, list(shape), dt, kind="ExternalInput").ap()

    def scr(name, shape, dt):
        return nc.dram_tensor(name, list(shape), dt, kind=kinds.get(name, "Internal")).ap()
    C.rw_w2 = din("rw_w2", [2, 2, 32, G])
    C.rw_a2 = din("rw_a2", [2, 2, 32, G])
    C.rw_g2 = din("rw_g2", [2, 64, G])
    C.rw_masks = din("rw_masks", [2, 128, 256])
    C.blk = din("blk", [128, 128])
    C.prep = scr("rw_prep", [2, 5, G, T], BF16)
    C.egc = scr("rw_egc", [2, G, NCH], F32)
    C.Yd = scr("rw_Y", [2, T, G], F32)


def phase_R1(C, l, e):
    kb = C.kb
    kb.begin()
    fwd = (e == 0)
    TB = 512
    NB = T // TB
    NC_ = TB // 128
    ppt = kb.alloc("ppt", [128, NPP], F32)
    kb.dma('sp', ppt[:], C.pp[l], w=['ppt'])
    omu = kb.alloc("omu", [128, 7], F32)
    omka = kb.alloc("omka", [128, 2], F32)
    mu0 = PP[f'mu{e}']
    kb.op('dve', lambda e_: e_.tensor_scalar(omu[:], ppt[:, mu0:mu0 + 7], -1.0, 1.0, ALU.mult, ALU.add), r=['ppt'], w=['omu'])
    kb.op('dve', lambda e_: e_.tensor_scalar(omka[:], ppt[:, PP['k_a']:PP['k_a'] + 2], -1.0, 1.0, ALU.mult, ALU.add), r=['ppt'], w=['omka'])
    w2z = kb.alloc("w2z", [64, G], F32)
    a2z = kb.alloc("a2z", [64, G], F32)
    blk = kb.alloc("blkf", [128, 128], F32)
    kb.op('dve', lambda e_: e_.memset(w2z[:], 0.0), w=['w2z'])
    kb.op('dve', lambda e_: e_.memset(a2z[:], 0.0), w=['a2z'])
    kb.dma('sp', w2z[0:32, :], C.rw_w2[l, e], w=['w2z'])
    kb.dma('sp', a2z[32:64, :], C.rw_a2[l, e], w=['a2z'])
    kb.dma('sp', blk[:], C.blk[:, :], w=['blkf'])
    egc = kb.alloc("egc", [128, 2, NCH], F32)
    PW = 192

    class Buf:
        def __init__(self, name, shape, dt, n=2, zero=False):
            self.t = [kb.alloc(f"{name}{i}", shape, dt) for i in range(n)]
            self.name = name
            self.n = n
            if zero:
                for i, t_ in enumerate(self.t):
                    kb.op('dve', lambda e_, t_=t_: e_.memset(t_[:], 0.0), w=[(name, i)])

        def get(self, it):
            return self.t[it % self.n], (self.name, it % self.n)

    Zx = {n: Buf(f"Zx{n}", [128, TB + 1], F32, n=3) for n in ('r', 'k', 'v', 'l')}
    tmpB = Buf("r1tmp", [128, TB], F32, n=3)
    zdB = {n: Buf(f"zd{n}", [128, TB], F32, n=3) for n in ('r', 'k', 'v', 'l')}
    tlB = Buf("tl", [64, TB], F32, n=3)
    sigB = Buf("sig", [128, TB], F32, n=3)
    asgB = Buf("asg", [128, TB], F32, n=3)
    scAB = Buf("scA", [128, NC_, PW], F32, n=3, zero=True)
    scBB = Buf("scB", [128, NC_, PW], F32, n=3, zero=True)
    E1B = Buf("E1", [128, TB], F32)
    E2B = Buf("E2", [128, TB], F32)
    E3B = Buf("E3", [128, TB], F32)
    kkB = Buf("kk", [128, TB], F32, n=3)
    kk2B = Buf("kk2", [128, TB], F32, n=3)
    rnB = Buf("rn", [128, TB], F32, n=3)
    facB = Buf("fac", [128, TB], F32)
    ob = {n: Buf(f"ob{n}", [128, TB], BF16) for n in ('a', 'r', 'k', 'b', 'v')}
    psw = [kb.palloc(f"psw{i}", [128, 512], F32) for i in range(2)]
    psa = [kb.palloc(f"psa{i}", [128, 512], F32) for i in range(2)]
    pss = [kb.palloc(f"pss{i}", [128, 512], F32) for i in range(2)]
    if fwd:
        cen = slice(1, TB + 1)
        sh = slice(0, TB)
        dat = slice(64, 192)
    else:
        cen = slice(0, TB)
        sh = slice(1, TB + 1)
        dat = slice(0, 128)
    nmix_box = [0]

    def load(name, row0, itn, tb):
        t0 = tb * TB
        z, key = Zx[name].get(itn)
        rows = C.zT_rw[row0:row0 + 128, :]
        if fwd:
            if tb == 0:
                kb.op('dve', lambda e_, z=z: e_.memset(z[:, 0:1], 0.0), w=[key])
                kb.dma('sp', z[:, 1:TB + 1], rows[:, 0:TB], w=[key])
            else:
                kb.dma('sp', z[:, :], rows[:, t0 - 1:t0 + TB], w=[key])
        else:
            if tb == NB - 1:
                kb.op('dve', lambda e_, z=z: e_.memset(z[:, TB:TB + 1], 0.0), w=[key])
                kb.dma('sp', z[:, 0:TB], rows[:, t0:T], w=[key])
            else:
                kb.dma('sp', z[:, :], rows[:, t0:t0 + TB + 1], w=[key])
        return z, key

    def mix(z, zkey, out, okey, mucol):
        tmp, tkey = tmpB.get(nmix_box[0])
        nmix_box[0] += 1
        kb.op('act', lambda e_: e_.activation(out=tmp[:], in_=z[:, cen], func=AF.Copy, scale=omu[:, mucol:mucol + 1]),
              r=[zkey, 'omu'], w=[tkey])
        kb.op('dve', lambda e_: e_.scalar_tensor_tensor(out=out[:], in0=z[:, sh], scalar=ppt[:, mu0 + mucol:mu0 + mucol + 1],
                                                        in1=tmp[:], op0=ALU.mult, op1=ALU.add),
              r=[zkey, 'ppt', tkey], w=[okey])

    def stage_X(it):
        tb, hp = it // 2, it % 2
        if hp == 0:
            z, zk = load('l', 768, tb, tb)
            zdl, zdlk = zdB['l'].get(tb)
            mix(z, zk, zdl, zdlk, 6)
            tl, tlk = tlB.get(tb)
            kb.op('act', lambda e_: e_.activation(out=tl[0:32, :], in_=zdl[0:32, :], func=AF.Tanh), r=[zdlk], w=[tlk])
            kb.op('act', lambda e_: e_.activation(out=tl[32:64, :], in_=zdl[32:64, :], func=AF.Copy), r=[zdlk], w=[tlk])
        tl, tlk = tlB.get(tb)
        chs = slice(hp * 128, (hp + 1) * 128)
        zr, zrk = load('r', hp * 128, it, tb)
        zk_, zkk = load('k', 256 + hp * 128, it, tb)
        zv, zvk = load('v', 512 + hp * 128, it, tb)
        zdr, zdrk = zdB['r'].get(it)
        zdk, zdkk = zdB['k'].get(it)
        zdv, zdvk = zdB['v'].get(it)
        mix(zr, zrk, zdr, zdrk, hp)
        mix(zk_, zkk, zdk, zdkk, 2 + hp)
        mix(zv, zvk, zdv, zdvk, 4 + hp)
        sig, sigk = sigB.get(it)
        asg, asgk = asgB.get(it)
        q = it % 2
        kb.op('pe', lambda e_: e_.matmul(psw[q][:], w2z[:, chs], tl[:], start=True, stop=True), r=['w2z', tlk], w=[('psw', q)])
        kb.op('pe', lambda e_: e_.matmul(psa[q][:], a2z[:, chs], tl[:], start=True, stop=True), r=['a2z', tlk], w=[('psa', q)])
        w0c = PP[f'w0_{e}'] + hp
        a0c = PP[f'a0_{e}'] + hp
        kb.op('act', lambda e_: e_.activation(out=sig[:], in_=psw[q][:], func=AF.Sigmoid, bias=ppt[:, w0c:w0c + 1]),
              r=[('psw', q), 'ppt'], w=[sigk])
        kb.op('act', lambda e_: e_.activation(out=asg[:], in_=psa[q][:], func=AF.Sigmoid, bias=ppt[:, a0c:a0c + 1]),
              r=[('psa', q), 'ppt'], w=[asgk])
        scA, scAk = scAB.get(it)
        kb.op('act', lambda e_: e_.activation(out=scA[:, :, dat], in_=sig[:].rearrange("p (c t) -> p c t", t=128), func=AF.Copy, scale=-E05),
              r=[sigk], w=[scAk])
        kkc = PP['k_k'] + hp
        kk, kkk = kkB.get(it)
        kk2, kk2k = kk2B.get(it)
        rn, rnk = rnB.get(it)
        kb.op('dve', lambda e_: e_.tensor_scalar(kk[:], zdk[:], ppt[:, kkc:kkc + 1], None, ALU.mult), r=[zdkk, 'ppt'], w=[kkk])
        kb.op('act', lambda e_: e_.activation(out=kk2[:], in_=zdk[:], func=AF.Square, scale=ppt[:, kkc:kkc + 1]), r=[zdkk, 'ppt'], w=[kk2k])
        kb.op('pe', lambda e_: e_.matmul(pss[q][:], blk[:], kk2[:], start=True, stop=True), r=['blkf', kk2k], w=[('pss', q)])
        kb.op('dve', lambda e_: e_.tensor_scalar(rn[:], pss[q][:], 1e-12, None, ALU.max), r=[('pss', q)], w=[rnk])

    def stage_Y(it):
        tb, hp = it // 2, it % 2
        t0 = tb * TB
        zdr, zdrk = zdB['r'].get(it)
        zdk, zdkk = zdB['k'].get(it)
        zdv, zdvk = zdB['v'].get(it)
        asg, asgk = asgB.get(it)
        scA, scAk = scAB.get(it)
        scB, scBk = scBB.get(it)
        kk, kkk = kkB.get(it)
        kk2, kk2k = kk2B.get(it)
        rn, rnk = rnB.get(it)
        src, dst, sk, dk = scA, scB, scAk, scBk
        for d in (1, 2, 4, 8, 16, 32, 64):
            if fwd:
                shd = slice(64 - d, 192 - d)
            else:
                shd = slice(d, 128 + d)
            kb.op('dve', lambda e_, src=src, dst=dst, shd=shd: e_.tensor_tensor(out=dst[:, :, dat], in0=src[:, :, dat], in1=src[:, :, shd],
                                                                               op=ALU.add),
                  r=[sk], w=[dk])
            yield
            src, dst, sk, dk = dst, src, dk, sk
        ci, cik = src, sk
        if fwd:
            cex = slice(63, 191)
            gcol = 191
        else:
            cex = slice(1, 129)
            gcol = 0
        v3 = lambda tle: tle[:].rearrange("p (c t) -> p c t", t=128)
        E1, E1k = E1B.get(it)
        E2, E2k = E2B.get(it)
        E3, E3k = E3B.get(it)
        fac, fack = facB.get(it)
        kb.op('act', lambda e_: e_.activation(out=v3(E1), in_=ci[:, :, dat], func=AF.Exp), r=[cik], w=[E1k])
        yield
        kb.op('act', lambda e_: e_.activation(out=v3(E2), in_=ci[:, :, dat], func=AF.Exp, scale=-1.0), r=[cik], w=[E2k])
        yield
        kb.op('act', lambda e_: e_.activation(out=v3(E3), in_=ci[:, :, cex], func=AF.Exp), r=[cik], w=[E3k])
        yield
        kb.op('act', lambda e_: e_.activation(out=egc[:, hp, tb * NC_:(tb + 1) * NC_], in_=ci[:, :, gcol], func=AF.Exp),
              r=[cik], w=[('egc', hp)])
        yield
        kb.op('act', lambda e_: e_.activation(out=rn[:], in_=rn[:], func=AF.Sqrt), r=[rnk], w=[rnk])
        yield
        kb.op('dve', lambda e_: e_.reciprocal(rn[:], rn[:]), r=[rnk], w=[rnk])
        yield
        kb.op('dve', lambda e_: e_.tensor_tensor(out=kk[:], in0=kk[:], in1=rn[:], op=ALU.mult), r=[kkk, rnk], w=[kkk])
        yield
        kac = PP['k_a'] + hp
        kb.op('act', lambda e_: e_.activation(out=fac[:], in_=asg[:], func=AF.Identity, scale=ppt[:, kac:kac + 1], bias=omka[:, hp:hp + 1]),
              r=[asgk, 'ppt', 'omka'], w=[fack])
        yield
        kb.op('dve', lambda e_: e_.tensor_tensor(out=fac[:], in0=fac[:], in1=zdk[:], op=ALU.mult), r=[fack, zdkk], w=[fack])
        yield
        oa, oak = ob['a'].get(it)
        ok_, okk = ob['k'].get(it)
        ob_, obk = ob['b'].get(it)
        or_, ork = ob['r'].get(it)
        ov, ovk = ob['v'].get(it)
        kb.op('dve', lambda e_: e_.scalar_tensor_tensor(out=oa[:], in0=kk[:], scalar=-1.0, in1=E3[:], op0=ALU.mult, op1=ALU.mult),
              r=[kkk, E3k], w=[oak])
        yield
        kb.op('pool', lambda e_: e_.tensor_tensor(out=ok_[:], in0=fac[:], in1=E2[:], op=ALU.mult), r=[fack, E2k], w=[okk])
        yield
        kb.op('dve', lambda e_: e_.tensor_tensor(out=kk2[:], in0=kk[:], in1=asg[:], op=ALU.mult), r=[kkk, asgk], w=[kk2k])
        yield
        kb.op('dve', lambda e_: e_.tensor_tensor(out=ob_[:], in0=kk2[:], in1=E2[:], op=ALU.mult), r=[kk2k, E2k], w=[obk])
        yield
        kb.op('pool', lambda e_: e_.tensor_tensor(out=or_[:], in0=zdr[:], in1=E1[:], op=ALU.mult), r=[zdrk, E1k], w=[ork])
        yield
        kb.op('act', lambda e_: e_.activation(out=ov[:], in_=zdv[:], func=AF.Copy), r=[zdvk], w=[ovk])
        yield
        for qi, (tile_, key_) in enumerate(((oa, oak), (or_, ork), (ok_, okk), (ob_, obk), (ov, ovk))):
            kb.dma('sp', C.prep[e, qi, hp * 128:(hp + 1) * 128, t0:t0 + TB], tile_[:], r=[key_])
            yield

    NIT = NB * 2
    LA = 2
    for it in range(min(LA, NIT)):
        stage_X(it)
    for it in range(0, NIT, 2):
        gens = [stage_Y(it), stage_Y(it + 1)]
        while gens:
            for g_ in list(gens):
                try:
                    next(g_)
                except StopIteration:
                    gens.remove(g_)
        for it2 in (it + LA, it + LA + 1):
            if it2 < NIT:
                stage_X(it2)
    for hp in range(2):
        kb.dma('sp', C.egc[e, hp * 128:(hp + 1) * 128, :], egc[:, hp, :], r=[('egc', hp)])
    kb.end()


def phase_R2(C, l, e, nchunks=NCH):
    kb = C.kb
    kb.begin()
    fwd = (e == 0)
    names = ('a', 'r', 'k', 'b', 'v')
    pre = {}
    for qi, n_ in enumerate(names):
        pre[n_] = kb.alloc(f"pre_{n_}", [128, 2, T], BF16)
        for hp in range(2):
            kb.dma('sp', pre[n_][:, hp, :], C.prep[e, qi, hp * 128:(hp + 1) * 128, :], w=[('pre', n_)])
    egc = kb.alloc("egc2", [128, 2, NCH], F32)
    for hp in range(2):
        kb.dma('sp', egc[:, hp, :], C.egc[e, hp * 128:(hp + 1) * 128, :], w=['egc2'])
    mask = kb.alloc("maskLQ", [128, 256], BF16)
    kb.dma('pool', mask[:], C.rw_masks[e], w=['mask'])
    maskN = kb.alloc("maskN", [128, 128], BF16)
    kb.dma('pool', maskN[:], C.rw_masks[1 - e, :, 0:128], w=['maskN'])
    kzb = [kb.alloc(f"kzb{i}", [128, 4, 128], BF16) for i in range(2)]
    bzb = [kb.alloc(f"bzb{i}", [128, 4, 128], BF16) for i in range(2)]
    azb = [kb.alloc(f"azb{i}", [128, 4, 128], BF16) for i in range(2)]
    for i in range(2):
        kb.op('dve', lambda e_, i=i: e_.memset(kzb[i][:], 0.0), w=[('kzb', i)])
        kb.op('dve', lambda e_, i=i: e_.memset(bzb[i][:], 0.0), w=[('bzb', i)])
        kb.op('dve', lambda e_, i=i: e_.memset(azb[i][:], 0.0), w=[('azb', i)])
    khat = [kb.alloc(f"khat{i}", [128, 2, 128], BF16) for i in range(2)]
    bhat = [kb.alloc(f"bhat{i}", [128, 2, 128], BF16) for i in range(2)]
    DG = [kb.alloc(f"DG{i}", [128, 2, 128], BF16) for i in range(2)]
    tokS = kb.alloc("tokS", [128, 2, 4, 128], BF16)
    LQA = kb.alloc("LQA", [128, 4, 256], BF16)
    LQB = kb.alloc("LQB", [128, 4, 256], BF16)
    PPb = [kb.alloc(f"PPb{i}", [128, 4, 2, 128], BF16) for i in range(3)]
    TT = [kb.alloc(f"TT{i}", [128, 4, 128], BF16) for i in range(2)]
    Wsb = kb.alloc("Wsb", [128, 4, 64], BF16)
    AV = kb.alloc("AV", [128, 4, 128], BF16)
    RT = kb.alloc("RT", [64, 4, 128], BF16)
    MT = kb.alloc("MT", [64, 4, 64], BF16)
    H = [kb.alloc(f"H{i}", [64, 4, 64], BF16) for i in range(2)]
    yst = [kb.alloc(f"ryst{i}", [128, 4, 64], F32) for i in range(2)]
    kb.op('dve', lambda e_: e_.memset(H[0][:], 0.0), w=[('H', 0)])
    pb = {i: kb.palloc(f"pb{i}", [128, 512], F32) for i in (0, 1, 2, 3, 6, 7)}
    tokT_t = kb.palloc("tokT", [128, 2, 4, 128], BF16)
    psW_t = kb.palloc("psW", [128, 4, 64], F32)
    identb = C.identb
    idb4 = identb[:, :].unsqueeze(1).to_broadcast([128, 4, 128])
    idb2 = identb[:, :].unsqueeze(1).to_broadcast([128, 2, 128])

    tokT = tokT_t[:]
    psA = [pb[hp][:, :].rearrange("p (u c) -> p u c", u=2) for hp in range(2)]
    psB = [pb[2 + hp][:, :].rearrange("p (u c) -> p u c", u=2) for hp in range(2)]
    psL = pb[6][:, :].rearrange("p (u c) -> p u c", u=4)
    psW = psW_t[:]
    psP = [pb[hp][:, :].rearrange("p (u k c) -> p u k c", u=2, k=2) for hp in range(2)]
    psT = pb[2][:, :].rearrange("p (u c) -> p u c", u=4)
    psAV = pb[3][:, :].rearrange("p (u c) -> p u c", u=4)
    psR = pb[6][0:64, :].rearrange("p (u c) -> p u c", u=4)
    psY = pb[7][:, 0:256].rearrange("p (u c) -> p u c", u=4)
    psM = psW_t[0:64, :, :]
    psN = pb[7][0:64, 256:512].rearrange("p (u c) -> p u c", u=4)
    K_ = lambda i: ('pb', i)

    def stage_a(n):
        c = n if fwd else NCH - 1 - n
        cs = slice(c * 128, (c + 1) * 128)
        i = n % 2
        for h2 in range(2):
            hs = slice(h2 * 64, (h2 + 1) * 64)
            kb.op('pool', lambda e_, hs=hs, h2=h2: e_.tensor_copy(out=kzb[i][hs, h2::2, :], in_=pre['k'][hs, :, cs]),
                  r=[('pre', 'k')], w=[('kzb', i)])
            kb.op('pool', lambda e_, hs=hs, h2=h2: e_.tensor_copy(out=bzb[i][hs, h2::2, :], in_=pre['b'][hs, :, cs]),
                  r=[('pre', 'b')], w=[('bzb', i)])
            kb.op('pool', lambda e_, hs=hs, h2=h2: e_.tensor_copy(out=azb[i][hs, h2::2, :], in_=pre['a'][hs, :, cs]),
                  r=[('pre', 'a')], w=[('azb', i)])
        gbc = egc[:, :, c:c + 1].to_broadcast([128, 2, 128])
        kb.op('pool', lambda e_: e_.tensor_tensor(out=khat[i][:], in0=pre['k'][:, :, cs], in1=gbc, op=ALU.mult),
              r=[('pre', 'k'), 'egc2'], w=[('khat', i)])
        kb.op('pool', lambda e_: e_.tensor_tensor(out=bhat[i][:], in0=pre['b'][:, :, cs], in1=gbc, op=ALU.mult),
              r=[('pre', 'b'), 'egc2'], w=[('bhat', i)])
        kb.op('pool', lambda e_: e_.tensor_tensor(out=DG[i][:], in0=idb2, in1=gbc, op=ALU.mult),
              r=['identb', 'egc2'], w=[('DG', i)])

    stage_a(0)

    def chunk_body(n, cur):
        c = n if fwd else NCH - 1 - n
        cs = slice(c * 128, (c + 1) * 128)
        i = n % 2
        for hp in range(2):
            srcs = [(pre['a'][:, hp, cs], ('pre', 'a')), (khat[i][:, hp, :], ('khat', i)), (bhat[i][:, hp, :], ('bhat', i)),
                    (pre['v'][:, hp, cs], ('pre', 'v'))]
            for q, (sap, skey) in enumerate(srcs):
                kb.op('pe', lambda e_, hp=hp, q=q, sap=sap: e_.transpose(tokT[:, hp, q, :], sap, identb[:]),
                      r=[skey, 'identb'], w=['tokT'])
        kb.op('act', lambda e_: e_.activation(out=tokS[:], in_=tokT, func=AF.Copy), r=['tokT'], w=['tokS'])
        for u in range(4):
            hp, h2 = u // 2, u % 2
            kb.op('pe', lambda e_, u=u, hp=hp, h2=h2: e_.matmul(psA[hp][:, h2, 0:128], kzb[i][:, u, :], pre['a'][:, hp, cs], start=True, stop=True),
                  r=[('kzb', i), ('pre', 'a')], w=[K_(hp)])
            kb.op('pe', lambda e_, u=u, hp=hp, h2=h2: e_.matmul(psA[hp][:, h2, 128:256], kzb[i][:, u, :], pre['r'][:, hp, cs], start=True, stop=True),
                  r=[('kzb', i), ('pre', 'r')], w=[K_(hp)])
        for u in range(4):
            hp, h2 = u // 2, u % 2
            kb.op('pe', lambda e_, u=u, hp=hp, h2=h2: e_.matmul(psB[hp][:, h2, 0:128], bzb[i][:, u, :], pre['a'][:, hp, cs], start=True, stop=True),
                  r=[('bzb', i), ('pre', 'a')], w=[K_(2 + hp)])
            kb.op('pe', lambda e_, u=u, hp=hp, h2=h2: e_.matmul(psB[hp][:, h2, 128:256], bzb[i][:, u, :], pre['r'][:, hp, cs], start=True, stop=True),
                  r=[('bzb', i), ('pre', 'r')], w=[K_(2 + hp)])
        for u in range(4):
            kb.op('pe', lambda e_, u=u: e_.matmul(psL[:, u, :], azb[i][:, u, :], pre['b'][:, u // 2, cs], start=True, stop=True),
                  r=[('azb', i), ('pre', 'b')], w=[K_(6)])
        mbc = mask[:, :].unsqueeze(1).to_broadcast([128, 2, 256])
        for hp in range(2):
            kb.op('dve', lambda e_, hp=hp: e_.tensor_tensor(out=LQB[:, 2 * hp:2 * hp + 2, :], in0=psB[hp], in1=mbc, op=ALU.mult),
                  r=[K_(2 + hp), 'mask'], w=['LQB'])
        kb.op('dve', lambda e_: e_.tensor_tensor(out=PPb[0][:, :, 0, :], in0=psL, in1=maskN[:, :].unsqueeze(1).to_broadcast([128, 4, 128]),
                                                 op=ALU.mult),
              r=[K_(6), 'maskN'], w=[('PPb', 0)])
        kb.op('dve', lambda e_: e_.tensor_tensor(out=TT[0][:], in0=LQB[:, :, 0:128], in1=idb4, op=ALU.add),
              r=['LQB', 'identb'], w=[('TT', 0)])
        for hp in range(2):
            kb.op('dve', lambda e_, hp=hp: e_.tensor_tensor(out=LQA[:, 2 * hp:2 * hp + 2, :], in0=psA[hp], in1=mbc, op=ALU.mult),
                  r=[K_(hp), 'mask'], w=['LQA'])
        if n + 1 < nchunks:
            stage_a(n + 1)

        def Pv(k, u):
            return PPb[k % 3][:, u, 0, :]

        def PTv(k, u):
            if k == 0:
                return LQB[:, u, 0:128]
            return PPb[k % 3][:, u, 1, :]

        def pkeys(k):
            return [('PPb', k % 3)] + (['LQB'] if k == 0 else [])

        def stage_P(k):
            for u in range(4):
                hp, h2 = u // 2, u % 2
                kb.op('pe', lambda e_, u=u, hp=hp, h2=h2: e_.matmul(psP[hp][:, h2, 0, :], PTv(k - 1, u), Pv(k - 1, u), start=True, stop=True),
                      r=pkeys(k - 1), w=[K_(hp)])
                if k < 6:
                    kb.op('pe', lambda e_, u=u, hp=hp, h2=h2: e_.matmul(psP[hp][:, h2, 1, :], Pv(k - 1, u), PTv(k - 1, u), start=True, stop=True),
                          r=pkeys(k - 1), w=[K_(hp)])
            for hp in range(2):
                eng = 'act' if hp == 0 else 'dve'
                if k < 6:
                    o_, i_ = PPb[k % 3][:, 2 * hp:2 * hp + 2, :, :], psP[hp]
                else:
                    o_, i_ = PPb[k % 3][:, 2 * hp:2 * hp + 2, 0, :], psP[hp][:, :, 0, :]
                if eng == 'act':
                    kb.op('act', lambda e_, o_=o_, i_=i_: e_.activation(out=o_, in_=i_, func=AF.Copy), r=[K_(hp)], w=[('PPb', k % 3)])
                else:
                    kb.op('dve', lambda e_, o_=o_, i_=i_: e_.tensor_copy(out=o_, in_=i_), r=[K_(hp)], w=[('PPb', k % 3)])

        def stage_T(k):
            pv, nx = (k - 1) % 2, k % 2
            for u in range(4):
                kb.op('pe', lambda e_, u=u: e_.matmul(psT[:, u, :], Pv(k, u), TT[pv][:, u, :], start=True, stop=False),
                      r=[('PPb', k % 3), ('TT', pv)], w=[K_(2)])
                kb.op('pe', lambda e_, u=u: e_.matmul(psT[:, u, :], identb[:], TT[pv][:, u, :], start=False, stop=True),
                      r=['identb', ('TT', pv)], w=[K_(2)])
            if k % 2 == 0:
                kb.op('act', lambda e_: e_.activation(out=TT[nx][:], in_=psT, func=AF.Copy), r=[K_(2)], w=[('TT', nx)])
            else:
                kb.op('dve', lambda e_: e_.tensor_copy(out=TT[nx][:], in_=psT), r=[K_(2)], w=[('TT', nx)])

        stage_P(1)
        for k in range(2, 7):
            stage_P(k)
            stage_T(k - 1)
        stage_T(6)
        TTf = TT[0]
        for u in range(4):
            hp, h2 = u // 2, u % 2
            hs = slice(h2 * 64, (h2 + 1) * 64)
            kb.op('pe', lambda e_, u=u, hp=hp, hs=hs: e_.matmul(psW[:, u, :], LQA[:, u, 0:128], tokS[:, hp, 3, hs], start=True, stop=True),
                  r=['LQA', 'tokS'], w=['psW'])
        kb.op('dve', lambda e_: e_.tensor_copy(out=Wsb[:], in_=psW), r=['psW'], w=['Wsb'])
        for u in range(4):
            hp, h2 = u // 2, u % 2
            hs = slice(h2 * 64, (h2 + 1) * 64)
            kb.op('pe', lambda e_, u=u, hp=hp, hs=hs: e_.matmul(psAV[:, u, 0:64], TTf[:, u, :], tokS[:, hp, 0, hs], start=True, stop=True),
                  r=[('TT', 0), 'tokS'], w=[K_(3)])
            kb.op('pe', lambda e_, u=u: e_.matmul(psAV[:, u, 64:128], TTf[:, u, :], Wsb[:, u, :], start=True, stop=True),
                  r=[('TT', 0), 'Wsb'], w=[K_(3)])
        kb.op('act', lambda e_: e_.activation(out=AV[:], in_=psAV, func=AF.Copy), r=[K_(3)], w=['AV'])
        for u in range(4):
            hp, h2 = u // 2, u % 2
            hs = slice(h2 * 64, (h2 + 1) * 64)
            kb.op('pe', lambda e_, u=u, hp=hp, hs=hs: e_.matmul(psR[:, u, :], identb[:, hs], pre['r'][:, hp, cs], start=True, stop=False),
                  r=['identb', ('pre', 'r')], w=[K_(6)])
            kb.op('pe', lambda e_, u=u: e_.matmul(psR[:, u, :], AV[:, u, 0:64], LQB[:, u, 128:256], start=False, stop=True),
                  r=['AV', 'LQB'], w=[K_(6)])
        kb.op('dve', lambda e_: e_.tensor_copy(out=RT[:], in_=psR), r=[K_(6)], w=['RT'])
        for u in range(4):
            hp, h2 = u // 2, u % 2
            hs = slice(h2 * 64, (h2 + 1) * 64)
            kb.op('pe', lambda e_, u=u, hp=hp, hs=hs: e_.matmul(psM[:, u, :], identb[:, hs], DG[i][:, hp, hs], start=True, stop=False),
                  r=['identb', ('DG', i)], w=['psW'])
            kb.op('pe', lambda e_, u=u, hp=hp, hs=hs: e_.matmul(psM[:, u, :], AV[:, u, 0:64], tokS[:, hp, 2, hs], start=False, stop=True),
                  r=['AV', 'tokS'], w=['psW'])
        kb.op('act', lambda e_: e_.activation(out=MT[:], in_=psM, func=AF.Copy), r=['psW'], w=['MT'])
        for u in range(4):
            hp, h2 = u // 2, u % 2
            hs = slice(h2 * 64, (h2 + 1) * 64)
            kb.op('pe', lambda e_, u=u, hp=hp, hs=hs: e_.matmul(psY[:, u, :], LQA[:, u, 128:256], tokS[:, hp, 3, hs], start=True, stop=False),
                  r=['LQA', 'tokS'], w=[K_(7)])
            kb.op('pe', lambda e_, u=u: e_.matmul(psY[:, u, :], LQB[:, u, 128:256], AV[:, u, 64:128], start=False, stop=False),
                  r=['LQB', 'AV'], w=[K_(7)])
            kb.op('pe', lambda e_, u=u, cur=cur: e_.matmul(psY[:, u, :], RT[:, u, :], H[cur][:, u, :], start=False, stop=True),
                  r=['RT', ('H', cur)], w=[K_(7)])
        ys = n % 2
        kb.op('dve', lambda e_, ys=ys: e_.tensor_copy(out=yst[ys][:], in_=psY), r=[K_(7)], w=[('ryst', ys)])
        kb.dma('sp', C.Yd[e, c * 128:(c + 1) * 128, :], yst[ys][:].rearrange("p u c -> p (u c)"), r=[('ryst', ys)])
        for u in range(4):
            hp, h2 = u // 2, u % 2
            hs = slice(h2 * 64, (h2 + 1) * 64)
            kb.op('pe', lambda e_, u=u, hp=hp, hs=hs: e_.matmul(psN[:, u, :], tokS[:, hp, 1, hs], tokS[:, hp, 3, hs], start=True, stop=False),
                  r=['tokS'], w=[K_(7)])
            kb.op('pe', lambda e_, u=u, hp=hp, hs=hs: e_.matmul(psN[:, u, :], tokS[:, hp, 2, hs], AV[:, u, 64:128], start=False, stop=False),
                  r=['tokS', 'AV'], w=[K_(7)])
            kb.op('pe', lambda e_, u=u, cur=cur: e_.matmul(psN[:, u, :], MT[:, u, :], H[cur][:, u, :], start=False, stop=True),
                  r=['MT', ('H', cur)], w=[K_(7)])
        kb.op('act', lambda e_, cur=cur: e_.activation(out=H[1 - cur][:], in_=psN, func=AF.Copy), r=[K_(7)], w=[('H', 1 - cur)])

    cur = 0
    for n in range(nchunks):
        chunk_body(n, cur)
        cur = 1 - cur
    kb.end()


def phase_R3(C, l):
    kb = C.kb
    kb.begin()
    ppt = kb.alloc("ppt3", [128, NPP], F32)
    kb.dma('sp', ppt[:], C.pp[l], w=['ppt'])
    blk = kb.alloc("blk3", [128, 128], F32)
    kb.dma('sp', blk[:], C.blk[:, :], w=['blk'])
    g2z = kb.alloc("g2z", [128, G], F32)
    kb.op('dve', lambda e_: e_.memset(g2z[:], 0.0), w=['g2z'])
    kb.dma('sp', g2z[64:128, :], C.rw_g2[l], w=['g2z'])
    ynT = kb.alloc("ynT", [128, 2, T], BF16)
    y0 = [kb.alloc(f"y0{i}", [128, 4, 64], F32) for i in range(2)]
    y1 = [kb.alloc(f"y1{i}", [128, 4, 64], F32) for i in range(2)]
    yc = [kb.alloc(f"ycn{i}", [128, 4, 64], F32) for i in range(2)]
    sq = kb.alloc("sq3", [128, 4, 64], F32)
    st = [kb.alloc(f"st3{i}", [128, 16], F32) for i in range(2)]
    ynb = [kb.alloc(f"ynb{i}", [128, G], BF16) for i in range(2)]
    ptt = [kb.palloc(f"ptt{i}", [128, 2, 128], BF16) for i in range(2)]
    for tt in range(NTT):
        i = tt % 2
        kb.dma('sp', y0[i][:].rearrange("p u c -> p (u c)"), C.Yd[0, tt * 128:(tt + 1) * 128, :], w=[('y0', i)])
        kb.dma('sp', y1[i][:].rearrange("p u c -> p (u c)"), C.Yd[1, tt * 128:(tt + 1) * 128, :], w=[('y1', i)])
        kb.op('pool', lambda e_, i=i: e_.tensor_tensor(out=y0[i][:], in0=y0[i][:], in1=y1[i][:], op=ALU.add),
              r=[('y0', i), ('y1', i)], w=[('y0', i)])
        s = st[i]
        sk = ('st3', i)
        kb.op('dve', lambda e_, i=i, s=s: e_.reduce_sum(out=s[:, 0:4], in_=y0[i][:], axis=AX.X), r=[('y0', i)], w=[sk])
        kb.op('dve', lambda e_, s=s: e_.tensor_scalar(s[:, 4:8], s[:, 0:4], 1.0 / 64, None, ALU.mult), r=[sk], w=[sk])
        kb.op('dve', lambda e_, i=i, s=s: e_.tensor_tensor(out=yc[i][:], in0=y0[i][:], in1=s[:, 4:8].unsqueeze(2).to_broadcast([128, 4, 64]),
                                                           op=ALU.subtract),
              r=[('y0', i), sk], w=[('ycn', i)])
        kb.op('pool', lambda e_, i=i: e_.tensor_tensor(out=sq[:], in0=yc[i][:], in1=yc[i][:], op=ALU.mult), r=[('ycn', i)], w=['sq3'])
        kb.op('dve', lambda e_, s=s: e_.reduce_sum(out=s[:, 8:12], in_=sq[:], axis=AX.X), r=['sq3'], w=[sk])
        kb.op('act', lambda e_, s=s: e_.activation(out=s[:, 12:16], in_=s[:, 8:12], func=AF.Sqrt, bias=C.cst[:, 1:2], scale=1.0 / 64),
              r=[sk, 'cst'], w=[sk])
        kb.op('dve', lambda e_, s=s: e_.reciprocal(s[:, 12:16], s[:, 12:16]), r=[sk], w=[sk])
        kb.op('dve', lambda e_, i=i, s=s: e_.tensor_tensor(out=ynb[i][:].rearrange("p (u c) -> p u c", u=4), in0=yc[i][:],
                                                           in1=s[:, 12:16].unsqueeze(2).to_broadcast([128, 4, 64]), op=ALU.mult),
              r=[('ycn', i), sk], w=[('ynb', i)])
        for hp in range(2):
            kb.op('pe', lambda e_, i=i, hp=hp: e_.transpose(ptt[i][:, hp, :], ynb[i][:, hp * 128:(hp + 1) * 128], C.identb[:]),
                  r=[('ynb', i), 'identb'], w=[('ptt', i)])
        kb.op('act', lambda e_, i=i, tt=tt: e_.activation(out=ynT[:, :, tt * 128:(tt + 1) * 128], in_=ptt[i][:], func=AF.Copy),
              r=[('ptt', i)], w=[('ynT', tt // 4)])
    z6 = [kb.alloc(f"z6{i}", [128, 512], F32) for i in range(2)]
    sgl = [kb.alloc(f"sgl{i}", [128, 512], F32) for i in range(2)]
    zr = [kb.alloc(f"zr3{i}", [128, 512], F32) for i in range(2)]
    zk = [kb.alloc(f"zk3{i}", [128, 512], F32) for i in range(2)]
    zv = [kb.alloc(f"zv3{i}", [128, 512], F32) for i in range(2)]
    rk = [kb.alloc(f"rk3{i}", [128, 512], F32) for i in range(2)]
    o1 = [kb.alloc(f"o13{i}", [128, 512], F32) for i in range(2)]
    bon = [kb.alloc(f"bon3{i}", [128, 512], F32) for i in range(2)]
    yo = kb.alloc("yo3", [128, 2, T], BF16)
    pbs = [kb.palloc(f"pbs{i}", [128, 512], F32) for i in range(2)]
    pg = [kb.palloc(f"pg{i}", [128, 512], F32) for i in range(2)]
    n = 0
    for g in range(8):
        gs = slice(g * 512, (g + 1) * 512)
        j = g % 2
        kb.dma('sp', z6[j][:], C.zT_rw[768:896, gs], w=[('z6', j)])
        kb.op('act', lambda e_, j=j: e_.activation(out=sgl[j][:], in_=z6[j][:], func=AF.Sigmoid), r=[('z6', j)], w=[('sgl', j)])
        for hp in range(2):
            i = n % 2
            n += 1
            kb.dma('sp', zr[i][:], C.zT_rw[hp * 128:(hp + 1) * 128, gs], w=[('zr3', i)])
            kb.dma('sp', zk[i][:], C.zT_rw[256 + hp * 128:256 + (hp + 1) * 128, gs], w=[('zk3', i)])
            kb.dma('sp', zv[i][:], C.zT_rw[512 + hp * 128:512 + (hp + 1) * 128, gs], w=[('zv3', i)])
            rkc = PP['r_k'] + hp
            kb.op('dve', lambda e_, i=i, rkc=rkc: e_.scalar_tensor_tensor(out=rk[i][:], in0=zr[i][:], scalar=ppt[:, rkc:rkc + 1], in1=zk[i][:],
                                                                            op0=ALU.mult, op1=ALU.mult),
                  r=[('zr3', i), ('zk3', i), 'ppt'], w=[('rk3', i)])
            kb.op('pe', lambda e_, i=i: e_.matmul(pbs[i][:], blk[:], rk[i][:], start=True, stop=True), r=['blk', ('rk3', i)], w=[('pbs', i)])
            kb.op('pe', lambda e_, i=i, j=j, hp=hp: e_.matmul(pg[i][:], g2z[:, hp * 128:(hp + 1) * 128], sgl[j][:], start=True, stop=True),
                  r=['g2z', ('sgl', j)], w=[('pg', i)])
            lw_, lb_ = PP['lnx_w'] + hp, PP['lnx_b'] + hp
            kb.op('dve', lambda e_, i=i, hp=hp, gs=gs, lw_=lw_, lb_=lb_: e_.tensor_scalar(
                o1[i][:], ynT[:, hp, gs], ppt[:, lw_:lw_ + 1], ppt[:, lb_:lb_ + 1], ALU.mult, ALU.add),
                r=[('ynT', g), 'ppt'], w=[('o13', i)])
            kb.op('dve', lambda e_, i=i: e_.tensor_tensor(out=bon[i][:], in0=pbs[i][:], in1=zv[i][:], op=ALU.mult),
                  r=[('pbs', i), ('zv3', i)], w=[('bon3', i)])
            kb.op('pool', lambda e_, i=i: e_.tensor_tensor(out=o1[i][:], in0=o1[i][:], in1=bon[i][:], op=ALU.add),
                  r=[('o13', i), ('bon3', i)], w=[('o13', i)])
            kb.op('dve', lambda e_, i=i, hp=hp, gs=gs: e_.tensor_tensor(out=yo[:, hp, gs], in0=pg[i][:], in1=o1[i][:], op=ALU.mult),
                  r=[('pg', i), ('o13', i)], w=[('yo3', hp)])
    for hp in range(2):
        kb.dma('sp', C.yT[256 + hp * 128:256 + (hp + 1) * 128, :], yo[:, hp, :], r=[('yo3', hp)])
    kb.end()


def build_program():
    nc = bass.Bass("TRN2", target_bir_lowering=False)
    C = declare(nc)
    declare2(C, nc)
    declare3(C, nc)
    declare4(C, nc)
    declare5(C, nc)
    declare6(C, nc)
    with ExitStack() as st:
        alloc_persistent(C, st)
        alloc_persistent2(C, st)
        phase_init(C)
        for l in range(2):
            xsrc = C.x if l == 0 else C.out
            moe = (l == 1)
            phase_mod(C, l)
            phase_A(C, l, xsrc)
            phase_NA(C, l)
            for e in range(2):
                phase_R1(C, l, e)
            for e in range(2):
                phase_R2(C, l, e)
            phase_R3(C, l)
            phase_DE(C, l)
            phase_F(C, l, xsrc, moe)
            phase_FFN(C, l, moe)
    return nc


def kernel(**inp):
    inp = {k: np.asarray(v) for k, v in inp.items()}
    B = inp["x"].shape[0]
    shared = {
        "ident": np.eye(128, dtype=np.float32),
        "pp": host_pack_pp(inp),
        "pool_rc": host_pool_rc(),
        "pool_w": np.ascontiguousarray(inp["pool_w"], dtype=np.float32),
        "router_wT": np.ascontiguousarray(inp["router_w"][0].T),
        "na_tab": np.stack([host_na_table(inp["na_rpb"][l]) for l in range(2)]),
        "rw_masks": host_rw_masks(),
        "blk": host_blk(),
        "ffn_w1": np.ascontiguousarray(inp["ffn_w1"][0]),
        "ffn_w3": np.ascontiguousarray(inp["ffn_w3"][0]),
        "ffn_w2": np.ascontiguousarray(inp["ffn_w2"][0]),
        "moe_w1": np.ascontiguousarray(inp["moe_w1"][0]),
        "moe_w3": np.ascontiguousarray(inp["moe_w3"][0]),
        "moe_w2": np.ascontiguousarray(inp["moe_w2"][0]),
    }
    for k in ["ada_w", "ada_b", "norm_g", "w_in", "w_out", "rw_w2", "rw_a2", "rw_g2"]:
        shared[k] = np.ascontiguousarray(inp[k], dtype=np.float32)
    in_maps = []
    for b in range(B):
        m = dict(shared)
        m["x"] = np.ascontiguousarray(inp["x"][b], dtype=np.float32)
        m["c_t"] = np.ascontiguousarray(inp["c"][b].reshape(8, 128).T, dtype=np.float32)
        in_maps.append(m)
    nc = build_program()
    res = run_bass_kernel_spmd(nc, in_maps, core_ids=list(range(B)))
    return np.stack([np.asarray(r["out"], dtype=np.float32) for r in res.results], axis=0)
```

```python
import os
import numpy as np
from contextlib import ExitStack
import concourse.bass as bass
import concourse.mybir as mybir
from concourse.bass_utils import run_bass_kernel_spmd


ENGS = ('pe', 'act', 'dve', 'pool', 'sp')
DMAQ = ('sp', 'act', 'pool')
NL = 8


class KB:
    def __init__(self, nc):
        self.nc = nc
        self.phase = 0

    def finish(self):
        pass

    def begin(self):
        self.stack = ExitStack()
        self.stack.__enter__()
        nc = self.nc
        self.phase += 1
        self.sem = {}
        for e in ENGS:
            self.sem[e] = nc.alloc_semaphore(name=f"s{self.phase}_{e}")
        for q in DMAQ:
            for l in range(NL):
                self.sem[('d', q, l)] = nc.alloc_semaphore(name=f"d{self.phase}_{q}{l}")
        self.cnt = {k: 0 for k in self.sem}
        self.seen = {e: {} for e in ENGS}
        self.last_w = {}
        self.rd = {}
        self.ops = {e: [] for e in ENGS}
        self.dma_i = {q: 0 for q in DMAQ}
        return self.stack

    def alloc(self, name, shape, dtype):
        return self.stack.enter_context(self.nc.sbuf_tensor(f"{name}_{self.phase}", list(shape), dtype))

    def palloc(self, name, shape, dtype):
        return self.stack.enter_context(self.nc.psum_tensor(f"{name}_{self.phase}", list(shape), dtype))

    def _deps(self, eng, r, w, is_dma=False):
        waits = {}
        seen = self.seen[eng]

        def need(sk, v, raw):
            if sk == eng and not is_dma and eng == 'pe':
                return
            if seen.get(sk, 0) < v and waits.get(sk, 0) < v:
                waits[sk] = v

        for k in r:
            lw = self.last_w.get(k)
            if lw:
                need(lw[0], lw[1], True)
        for k in w:
            lw = self.last_w.get(k)
            if lw:
                need(lw[0], lw[1], False)
            for sk, v in self.rd.get(k, {}).items():
                need(sk, v, False)
        for sk, v in waits.items():
            seen[sk] = v
        return list(waits.items())

    def _record(self, sk, v, r, w):
        for k in r:
            d = self.rd.setdefault(k, {})
            if d.get(sk, 0) < v:
                d[sk] = v
        for k in w:
            self.last_w[k] = (sk, v)
            self.rd[k] = {}

    def op(self, eng, fn, r=(), w=()):
        waits = self._deps(eng, r, w)
        self.cnt[eng] += 1
        v = self.cnt[eng]
        self._record(eng, v, r, w)
        self.ops[eng].append((waits, fn, (eng, 1)))

    def dma(self, q, out, in_, r=(), w=()):
        lane = self.dma_i[q] % NL
        self.dma_i[q] += 1
        sk = ('d', q, lane)
        waits = self._deps(q, r, w, is_dma=True)
        prev = self.cnt[sk]
        if prev > 0 and self.seen[q].get(sk, 0) < prev:
            waits.append((sk, prev))
            self.seen[q][sk] = prev
        v = prev + 16
        self.cnt[sk] = v
        self._record(sk, v, r, w)
        self.ops[q].append((waits, (lambda e, o=out, i=in_: e.dma_start(out=o, in_=i)), (sk, 16)))

    def end(self):
        final = dict(self.cnt)
        sem = self.sem
        ops = self.ops

        def mk(e):
            def body(eo):
                for waits, fn, inc in ops[e]:
                    for sk, v in waits:
                        eo.wait_ge(sem[sk], v)
                    ins = fn(eo)
                    ins.then_inc(sem[inc[0]], inc[1])
                for sk, v in final.items():
                    if v > 0:
                        eo.wait_ge(sem[sk], v)
            return body

        with self.nc.Block() as block:
            block.tensor(mk('pe'))
            block.scalar(mk('act'))
            block.vector(mk('dve'))
            block.gpsimd(mk('pool'))
            block.sync(mk('sp'))
        self.stack.__exit__(None, None, None)
        self.nc.all_engine_barrier()
        self.nc.clear_and_free_semaphores(list(self.sem.values()))
        self.nc.all_engine_barrier()
        self.ops = None


F32 = mybir.dt.float32
BF16 = mybir.dt.bfloat16
AF = mybir.ActivationFunctionType
ALU = mybir.AluOpType
AX = mybir.AxisListType

T = 4096
D = 1024
G = 256
INW = 2688
NTT = 32
RMS_EPS = 1e-6


class Ctx:
    pass


def declare(nc, kinds=None):
    kinds = kinds or {}
    C = Ctx()
    C.nc = nc
    C.kb = KB(nc)

    def din(name, shape, dt=F32):
        return nc.dram_tensor(name, list(shape), dt, kind="ExternalInput").ap()

    def scr(name, shape, dt):
        return nc.dram_tensor(name, list(shape), dt, kind=kinds.get(name, "Internal")).ap()

    C.x = din("x", [T, D])
    C.c_t = din("c_t", [128, 8])
    C.ada_w = din("ada_w", [2, D, 6 * D])
    C.ada_b = din("ada_b", [2, 6 * D])
    C.norm_g = din("norm_g", [2, 4, D])
    C.w_in = din("w_in", [2, D, INW])
    C.w_out = din("w_out", [2, D, D])
    C.ident = din("ident", [128, 128])
    C.out = nc.dram_tensor("out", [T, D], F32, kind="ExternalOutput").ap()
    C.zT_bf = scr("zT_bf", [INW, T], BF16)
    C.zT_rw = scr("zT_rw", [896, T], F32)
    C.vtok = scr("vtok", [T, G], BF16)
    C.yT = scr("yT", [D, T], BF16)
    C.h2T = scr("h2T", [D, T], BF16)
    return C


def alloc_persistent(C, stack):
    nc = C.nc
    C.modt = stack.enter_context(nc.sbuf_tensor("modt", [128, 6, D], F32))
    C.identb = stack.enter_context(nc.sbuf_tensor("identb", [128, 128], BF16))
    C.identf = stack.enter_context(nc.sbuf_tensor("identf", [128, 128], F32))
    C.cst = stack.enter_context(nc.sbuf_tensor("cst", [128, 8], F32))
    C.ones_f = stack.enter_context(nc.sbuf_tensor("ones_f", [128, 128], F32))
    C.ones_b = stack.enter_context(nc.sbuf_tensor("ones_b", [128, 128], BF16))


def phase_init(C):
    kb = C.kb
    kb.begin()
    kb.dma('sp', C.identf[:], C.ident[:, :], w=['identf'])
    kb.dma('pool', C.identb[:], C.ident[:, :], w=['identb'])
    kb.op('dve', lambda e: e.memset(C.cst[:, 0:1], RMS_EPS), w=['cst'])
    kb.op('dve', lambda e: e.memset(C.cst[:, 1:2], 64e-5), w=['cst'])
    kb.op('dve', lambda e: e.memset(C.cst[:, 2:3], 0.0), w=['cst'])
    kb.op('dve', lambda e: e.memset(C.cst[:, 3:4], 1.0), w=['cst'])
    kb.op('dve', lambda e: e.memset(C.ones_f[:], 1.0), w=['ones_f'])
    kb.op('dve', lambda e: e.memset(C.ones_b[:], 1.0), w=['ones_b'])
    kb.end()


def phase_mod(C, l):
    kb = C.kb
    nc = C.nc
    kb.begin()
    ct = kb.alloc("ct", [128, 8], F32)
    sc = kb.alloc("sc", [128, 8], F32)
    scb = kb.alloc("scb", [128, 8, 128], F32)
    adab = kb.alloc("adab", [1, 6 * D], F32)
    gbc = kb.alloc("gbc", [128, 4, D], F32)
    wblk = [kb.alloc(f"wblk{i}", [128, 8, 512], F32) for i in range(2)]
    ps = [kb.palloc(f"mps{i}", [128, 512], F32) for i in range(2)]

    kb.dma('sp', ct[:], C.c_t[:, :], w=['ct'])
    kb.dma('sp', adab[:], C.ada_b[l:l + 1, :], w=['adab'])
    for j in range(4):
        kb.dma('sp', gbc[:, j, :], C.norm_g[l, j, :].partition_broadcast(128), w=[('gbc', j)])
    kb.op('act', lambda e: e.activation(out=sc[:], in_=ct[:], func=AF.Silu), r=['ct'], w=['sc'])
    kb.op('dve', lambda e: e.tensor_copy(out=scb[:], in_=sc[:, 0:8].unsqueeze(2).to_broadcast([128, 8, 128])),
          r=['sc'], w=['scb'])
    wv = C.ada_w[l].rearrange("(k p) n -> p k n", p=128)
    sect = [(1, 'copy', None), (0, 'a', 0), (2, 'g', 1), (4, 'copy', None), (3, 'a', 2), (5, 'g', 3)]
    for nb in range(12):
        b = nb % 2
        kb.dma('sp', wblk[b][:], wv[:, :, nb * 512:(nb + 1) * 512], w=[('wblk', b)])
        for k in range(8):
            kb.op('pe', lambda e, b=b, k=k: e.matmul(ps[b][:], scb[:, k, :], wblk[b][:, k, :], start=(k == 0), stop=False),
                  r=['scb', ('wblk', b)], w=[('mps', b)])
        kb.op('pe', lambda e, b=b, nb=nb: e.matmul(ps[b][:], C.ones_f[0:1, :], adab[0:1, nb * 512:(nb + 1) * 512],
                                                   start=False, stop=True),
              r=['ones_f', 'adab'], w=[('mps', b)])
        s = nb // 2
        cols = slice((nb % 2) * 512, (nb % 2) * 512 + 512)
        slot, mode, gi = sect[s]
        dst = C.modt[:, slot, cols]
        if mode == 'copy':
            kb.op('act', lambda e, dst=dst, b=b: e.activation(out=dst, in_=ps[b][:], func=AF.Copy),
                  r=[('mps', b)], w=[('modt', slot)])
        elif mode == 'a':
            kb.op('dve', lambda e, dst=dst, b=b, gi=gi, cols=cols: e.scalar_tensor_tensor(
                out=dst, in0=ps[b][:], scalar=1.0, in1=gbc[:, gi, cols], op0=ALU.add, op1=ALU.mult),
                r=[('mps', b), ('gbc', gi)], w=[('modt', slot)])
        else:
            kb.op('dve', lambda e, dst=dst, b=b, gi=gi, cols=cols: e.tensor_tensor(
                out=dst, in0=ps[b][:], in1=gbc[:, gi, cols], op=ALU.mult),
                r=[('mps', b), ('gbc', gi)], w=[('modt', slot)])
    kb.end()


def emit_rstd(kb, C, ss, rstd, tag, n):
    kb.op('act', lambda e: e.activation(out=rstd[:, 0:n], in_=ss[:, 0:n], func=AF.Sqrt, bias=C.cst[:, 0:1], scale=1.0 / D),
          r=[(tag, 'ss'), 'cst'], w=[(tag, 'rstd')])
    kb.op('dve', lambda e: e.reciprocal(rstd[:, 0:n], rstd[:, 0:n]), r=[(tag, 'rstd')], w=[(tag, 'rstd')])


def phase_A(C, l, xsrc):
    kb = C.kb
    kb.begin()
    win = kb.alloc("win", [128, 8, INW], BF16)
    xin = [[kb.alloc(f"xin{b}_{j}", [128, D], F32) for j in range(4)] for b in range(2)]
    junk = kb.alloc("junk", [128, D], BF16)
    tmp = [kb.alloc(f"tmp{i}", [128, D], F32) for i in range(2)]
    hb = [kb.alloc(f"hb{i}", [128, D], BF16) for i in range(2)]
    hT = [kb.alloc(f"hT{i}", [128, 8, 512], BF16) for i in range(2)]
    ss = [kb.alloc(f"ss{i}", [128, 4], F32) for i in range(2)]
    rstd = [kb.alloc(f"rstd{i}", [128, 4], F32) for i in range(2)]
    stg_bf = [kb.alloc(f"stgb{i}", [128, 512], BF16) for i in range(3)]
    stg_rw = [kb.alloc(f"stgr{i}", [128, 512], F32) for i in range(2)]
    vst = [kb.alloc(f"vst{i}", [128, G], BF16) for i in range(2)]
    pt = [kb.palloc(f"pt{i}", [128, 8, 128], BF16) for i in range(2)]
    zp = [kb.palloc(f"zp{i}", [128, 512], F32) for i in range(3)]
    vp = [kb.palloc(f"vp{i}", [128, G], F32) for i in range(2)]

    wv = C.w_in[l].rearrange("(k p) n -> p k n", p=128)
    for k in range(8):
        kb.dma('pool', win[:, k, :], wv[:, k, :], w=[('win', k)])
    winkeys = [('win', k) for k in range(8)]
    A1 = C.modt[:, 0, :]
    B1 = C.modt[:, 1, :]
    nev = 0
    ntp = 0
    for g in range(8):
        b = g % 2
        for j in range(4):
            tt = g * 4 + j
            kb.dma('sp', xin[b][j][:], xsrc[tt * 128:(tt + 1) * 128, :], w=[('xin', b, j)])
            kb.op('act', lambda e, b=b, j=j: e.activation(out=junk[:], in_=xin[b][j][:], func=AF.Square,
                                                          accum_out=ss[b][:, j:j + 1]),
                  r=[('xin', b, j)], w=['junk', (('A', b), 'ss')])
        emit_rstd(kb, C, ss[b], rstd[b], ('A', b), 4)
        for j in range(4):
            i2 = j % 2
            kb.op('dve', lambda e, b=b, j=j, i2=i2: e.scalar_tensor_tensor(
                out=tmp[i2][:], in0=xin[b][j][:], scalar=rstd[b][:, j:j + 1], in1=A1, op0=ALU.mult, op1=ALU.mult),
                r=[('xin', b, j), (('A', b), 'rstd'), ('modt', 0)], w=[('tmp', i2)])
            kb.op('pool', lambda e, i2=i2: e.tensor_tensor(out=hb[i2][:], in0=tmp[i2][:], in1=B1, op=ALU.add),
                  r=[('tmp', i2), ('modt', 1)], w=[('hb', i2)])
            p = ntp % 2
            ntp += 1
            for k in range(8):
                kb.op('pe', lambda e, p=p, k=k, i2=i2: e.transpose(pt[p][:, k, :], hb[i2][:, k * 128:(k + 1) * 128], C.identb[:]),
                      r=[('hb', i2), 'identb'], w=[('pt', p)])
            kb.op('act', lambda e, p=p, b=b, j=j: e.activation(out=hT[b][:, :, j * 128:(j + 1) * 128], in_=pt[p][:], func=AF.Copy),
                  r=[('pt', p)], w=[('hT', b, j)])
        hkeys = [('hT', b, j) for j in range(4)]
        for j in range(4):
            tt = g * 4 + j
            q = j % 2
            for k in range(8):
                kb.op('pe', lambda e, q=q, k=k, b=b, j=j: e.matmul(vp[q][:], hT[b][:, k, j * 128:(j + 1) * 128], win[:, k, 512:768],
                                                                   start=(k == 0), stop=(k == 7)),
                      r=[('hT', b, j)] + winkeys, w=[('vp', q)])
            kb.op('dve', lambda e, q=q: e.tensor_copy(out=vst[q][:], in_=vp[q][:]), r=[('vp', q)], w=[('vst', q)])
            kb.dma('sp', C.vtok[tt * 128:(tt + 1) * 128, :], vst[q][:], r=[('vst', q)])
        for fc in range(21):
            z = nev % 3
            for k in range(8):
                kb.op('pe', lambda e, z=z, k=k, b=b, fc=fc: e.matmul(zp[z][:], win[:, k, fc * 128:(fc + 1) * 128], hT[b][:, k, :],
                                                                     start=(k == 0), stop=(k == 7)),
                      r=hkeys + winkeys, w=[('zp', z)])
            is_rw = 6 <= fc < 13
            eng = 'act' if nev % 2 == 0 else 'dve'
            if is_rw:
                s = fc % 2
                if eng == 'act':
                    kb.op('act', lambda e, z=z, s=s: e.activation(out=stg_rw[s][:], in_=zp[z][:], func=AF.Copy),
                          r=[('zp', z)], w=[('stgr', s)])
                else:
                    kb.op('dve', lambda e, z=z, s=s: e.tensor_copy(out=stg_rw[s][:], in_=zp[z][:]),
                          r=[('zp', z)], w=[('stgr', s)])
                kb.dma('sp', C.zT_rw[(fc - 6) * 128:(fc - 5) * 128, g * 512:(g + 1) * 512], stg_rw[s][:], r=[('stgr', s)])
            else:
                s = nev % 3
                if eng == 'act':
                    kb.op('act', lambda e, z=z, s=s: e.activation(out=stg_bf[s][:], in_=zp[z][:], func=AF.Copy),
                          r=[('zp', z)], w=[('stgb', s)])
                else:
                    kb.op('dve', lambda e, z=z, s=s: e.tensor_copy(out=stg_bf[s][:], in_=zp[z][:]),
                          r=[('zp', z)], w=[('stgb', s)])
                kb.dma('sp', C.zT_bf[fc * 128:(fc + 1) * 128, g * 512:(g + 1) * 512], stg_bf[s][:], r=[('stgb', s)])
            nev += 1
    kb.end()


PP = {}
_pp_items = [('pool_scale', 2), ('conv_w', 6), ('mu0', 7), ('mu1', 7), ('w0_0', 2), ('w0_1', 2), ('a0_0', 2), ('a0_1', 2),
             ('k_k', 2), ('k_a', 2), ('lnx_w', 2), ('lnx_b', 2), ('r_k', 2)]
_c = 0
for _n, _k in _pp_items:
    PP[_n] = _c
    _c += _k
NPP = _c


def host_pack_pp(inp):
    pp = np.zeros((2, 128, NPP), np.float32)

    def put(l, name, vec, off=0):
        n = vec.shape[0]
        nch = (n + 127) // 128
        for c in range(nch):
            seg = vec[c * 128:(c + 1) * 128]
            pp[l, :seg.shape[0], PP[name] + off + c] = seg
    for l in range(2):
        put(l, 'pool_scale', inp['pool_scale'][l])
        for k in range(3):
            put(l, 'conv_w', inp['conv_w'][l, k], off=2 * k)
        for e in range(2):
            put(l, f'mu{e}', inp['rw_mu'][l, e])
            put(l, f'w0_{e}', inp['rw_w0'][l, e])
            put(l, f'a0_{e}', inp['rw_a0'][l, e])
        put(l, 'k_k', inp['rw_k_k'][l])
        put(l, 'k_a', inp['rw_k_a'][l])
        put(l, 'lnx_w', inp['rw_lnx_w'][l])
        put(l, 'lnx_b', inp['rw_lnx_b'][l])
        put(l, 'r_k', inp['rw_r_k'][l].reshape(-1))
    return pp


def host_pool_rc():
    wins = (2, 4, 8, 16)
    t = np.arange(T)
    rc = np.zeros((2, 128, T), np.float32)
    for g, w in enumerate(wins):
        lo = np.clip(t - w // 2, 0, T)
        hi = np.clip(t - w // 2 + w, 0, T)
        r = (1.0 / (hi - lo)).astype(np.float32)
        rc[g // 2, (g % 2) * 64:(g % 2) * 64 + 64, :] = r[None, :]
    return rc


def declare2(C, nc):
    def din(name, shape, dt=F32):
        return nc.dram_tensor(name, list(shape), dt, kind="ExternalInput").ap()
    C.pp = din("pp", [2, 128, NPP])
    C.pool_rc = din("pool_rc", [2, 128, T])
    C.pool_w = din("pool_w", [2, 4, 64, 64])


def phase_DE(C, l):
    kb = C.kb
    kb.begin()
    L = T + 32
    ppt = kb.alloc("ppt", [128, NPP], F32)
    kb.dma('sp', ppt[:], C.pp[l], w=['ppt'])
    zt = [kb.alloc(f"zt{i}", [128, T], BF16) for i in range(8)]
    for i in range(8):
        kb.dma('sp', zt[i][:], C.zT_bf[(13 + i) * 128:(14 + i) * 128, :], w=[('zt', i)])
    rc = [kb.alloc(f"rc{i}", [128, T], F32) for i in range(2)]
    for i in range(2):
        kb.dma('sp', rc[i][:], C.pool_rc[i], w=[('rc', i)])
    pwb = [kb.alloc(f"pwb{i}", [128, 128], BF16) for i in range(2)]
    for ci in range(2):
        kb.op('dve', lambda e, ci=ci: e.memset(pwb[ci][:], 0.0), w=[('pwb', ci)])
        for h in range(2):
            kb.dma('pool', pwb[ci][h * 64:(h + 1) * 64, h * 64:(h + 1) * 64], C.pool_w[l, ci * 2 + h], r=[], w=[('pwb', ci)])
    upad = kb.alloc("upad", [128, L], F32)
    bufA = kb.alloc("bufA", [128, L], F32)
    bufB = kb.alloc("bufB", [128, L], F32)
    dT = kb.alloc("dT", [128, T], BF16)
    yst = [kb.alloc(f"yst{i}", [128, T], BF16) for i in range(2)]
    pp_ = [kb.palloc(f"pps{i}", [128, 512], F32) for i in range(2)]
    kb.op('dve', lambda e: e.memset(upad[:, 0:16], 0.0), w=['upad'])
    kb.op('dve', lambda e: e.memset(upad[:, 16 + T:L], 0.0), w=['upad'])
    for ci in range(2):
        kb.op('act', lambda e, ci=ci: e.activation(out=upad[:, 16:16 + T], in_=zt[ci][:], func=AF.Copy),
              r=[('zt', ci)], w=['upad'])
        kb.op('dve', lambda e: e.tensor_tensor(out=bufA[:, 1:L], in0=upad[:, 0:L - 1], in1=upad[:, 1:L], op=ALU.add),
              r=['upad'], w=['bufA'])
        kb.op('dve', lambda e: e.tensor_tensor(out=bufB[:, 2:L - 1], in0=bufA[:, 1:L - 2], in1=bufA[:, 3:L], op=ALU.add),
              r=['bufA'], w=['bufB'])
        if ci == 1:
            kb.op('dve', lambda e: e.tensor_tensor(out=bufA[:, 4:L - 3], in0=bufB[:, 2:L - 5], in1=bufB[:, 6:L - 1], op=ALU.add),
                  r=['bufB'], w=['bufA'])
            kb.op('dve', lambda e: e.tensor_tensor(out=bufB[:, 8:L - 7], in0=bufA[:, 4:L - 11], in1=bufA[:, 12:L - 3], op=ALU.add),
                  r=['bufA'], w=['bufB'])
        kb.op('pool', lambda e, ci=ci: e.tensor_tensor(out=bufA[0:64, 16:16 + T], in0=bufA[0:64, 16:16 + T], in1=rc[ci][0:64, :], op=ALU.mult),
              r=['bufA', ('rc', ci)], w=['bufA'])
        kb.op('dve', lambda e, ci=ci: e.tensor_tensor(out=bufB[64:128, 16:16 + T], in0=bufB[64:128, 16:16 + T], in1=rc[ci][64:128, :], op=ALU.mult),
              r=['bufB', ('rc', ci)], w=['bufB'])
        kb.op('pool', lambda e: e.tensor_tensor(out=dT[0:64, :], in0=bufA[0:64, 16:16 + T], in1=upad[0:64, 16:16 + T], op=ALU.subtract),
              r=['bufA', 'upad'], w=['dT'])
        kb.op('dve', lambda e: e.tensor_tensor(out=dT[64:128, :], in0=bufB[64:128, 16:16 + T], in1=upad[64:128, 16:16 + T], op=ALU.subtract),
              r=['bufB', 'upad'], w=['dT'])
        for nt in range(8):
            q = nt % 2
            kb.op('pe', lambda e, q=q, nt=nt, ci=ci: e.matmul(pp_[q][:], pwb[ci][:], dT[:, nt * 512:(nt + 1) * 512], start=True, stop=True),
                  r=[('pwb', ci), 'dT'], w=[('pps', q)])
            col = PP['pool_scale'] + ci
            kb.op('act', lambda e, q=q, nt=nt, ci=ci, col=col: e.activation(
                out=yst[ci][:, nt * 512:(nt + 1) * 512], in_=pp_[q][:], func=AF.Copy, scale=ppt[:, col:col + 1]),
                r=[('pps', q), 'ppt'], w=[('yst', ci)])
        kb.dma('sp', C.yT[512 + ci * 128:512 + (ci + 1) * 128, :], yst[ci][:], r=[('yst', ci)])
    up2 = upad
    acc = bufA
    yc = yst
    kb.op('dve', lambda e: e.memset(up2[:, 0:1], 0.0), w=['upad'])
    kb.op('dve', lambda e: e.memset(up2[:, T + 1:T + 2], 0.0), w=['upad'])
    for ci in range(2):
        bg, cg, hin = zt[2 + ci], zt[4 + ci], zt[6 + ci]
        kb.op('dve', lambda e, cg=cg, hin=hin: e.tensor_tensor(out=up2[:, 1:T + 1], in0=cg[:], in1=hin[:], op=ALU.mult),
              r=[('zt', 4 + ci), ('zt', 6 + ci)], w=['upad'])
        cw = PP['conv_w']
        kb.op('pool', lambda e, ci=ci, cw=cw: e.tensor_scalar(acc[:, 0:T], up2[:, 1:T + 1], ppt[:, cw + 2 + ci:cw + 3 + ci], None, ALU.mult),
              r=['upad', 'ppt'], w=['bufA'])
        kb.op('dve', lambda e, ci=ci, cw=cw: e.scalar_tensor_tensor(out=acc[:, 0:T], in0=up2[:, 0:T], scalar=ppt[:, cw + ci:cw + ci + 1],
                                                                    in1=acc[:, 0:T], op0=ALU.mult, op1=ALU.add),
              r=['upad', 'ppt', 'bufA'], w=['bufA'])
        kb.op('dve', lambda e, ci=ci, cw=cw: e.scalar_tensor_tensor(out=acc[:, 0:T], in0=up2[:, 2:T + 2], scalar=ppt[:, cw + 4 + ci:cw + 5 + ci],
                                                                    in1=acc[:, 0:T], op0=ALU.mult, op1=ALU.add),
              r=['upad', 'ppt', 'bufA'], w=['bufA'])
        kb.op('pool', lambda e, ci=ci, bg=bg: e.tensor_tensor(out=yc[ci][:], in0=acc[:, 0:T], in1=bg[:], op=ALU.mult),
              r=['bufA', ('zt', 2 + ci)], w=[('yst', ci)])
        kb.dma('sp', C.yT[768 + ci * 128:768 + (ci + 1) * 128, :], yc[ci][:], r=[('yst', ci)])
    kb.end()


def declare3(C, nc, kinds=None):
    kinds = kinds or {}
    C.router_wT = nc.dram_tensor("router_wT", [8, D], F32, kind="ExternalInput").ap()
    C.xmid = nc.dram_tensor("xmid", [T, D], F32, kind=kinds.get("xmid", "Internal")).ap()


def alloc_persistent2(C, stack):
    nc = C.nc
    C.gates = stack.enter_context(nc.sbuf_tensor("gates", [128, NTT, 8], F32))


def phase_F(C, l, xsrc, moe):
    kb = C.kb
    kb.begin()
    wout = kb.alloc("wout", [128, 8, D], BF16)
    wv = C.w_out[l].rearrange("(k p) n -> p k n", p=128)
    for k in range(8):
        kb.dma('pool', wout[:, k, :], wv[:, k, :], w=[('wout', k)])
    wkeys = [('wout', k) for k in range(8)]
    yTg = [kb.alloc(f"yTg{i}", [128, 8, 512], BF16) for i in range(2)]
    xin = [kb.alloc(f"fx{i}", [128, D], F32) for i in range(3)]
    tmp = [kb.alloc(f"ft{i}", [128, D], F32) for i in range(2)]
    xn = [kb.alloc(f"fxn{i}", [128, D], F32) for i in range(2)]
    h2 = [kb.alloc(f"fh{i}", [128, D], F32) for i in range(2)]
    h2b = [kb.alloc(f"fhb{i}", [128, D], BF16) for i in range(2)]
    junk = kb.alloc("fjunk", [128, D], BF16)
    ss = [kb.alloc(f"fss{i}", [128, 4], F32) for i in range(4)]
    rs = [kb.alloc(f"frs{i}", [128, 4], F32) for i in range(4)]
    h2Tg = [kb.alloc(f"h2Tg{i}", [128, 8, 512], BF16) for i in range(2)]
    yps = [kb.palloc(f"yps{i}", [128, D], F32) for i in range(3)]
    pt = [kb.palloc(f"fpt{i}", [128, 8, 128], BF16) for i in range(2)]
    G1 = C.modt[:, 2, :]
    A2 = C.modt[:, 3, :]
    B2 = C.modt[:, 4, :]
    if moe:
        rwb = kb.alloc("rwb", [128, 8, D], F32)
        for e_ in range(8):
            kb.dma('sp', rwb[:, e_, :], C.router_wT[e_, :].partition_broadcast(128), w=[('rwb', e_)])
        lg = [kb.alloc(f"lg{i}", [128, 8], F32) for i in range(2)]
        sm = [kb.alloc(f"sm{i}", [128, 16], F32) for i in range(2)]
        eq1 = [kb.alloc(f"eq1{i}", [128, 8], F32) for i in range(2)]
        eq2 = [kb.alloc(f"eq2{i}", [128, 8], F32) for i in range(2)]
        l2 = [kb.alloc(f"l2{i}", [128, 8], F32) for i in range(2)]
        rjunk = kb.alloc("rjunk", [128, D], BF16)
    xvd = C.yT.rearrange("(k p) t -> p k t", p=128)
    h2v = C.h2T.rearrange("(k p) t -> p k t", p=128)
    NB3 = 3

    def emit_load(g):
        b = g % 2
        kb.dma('sp', yTg[b][:], xvd[:, :, g * 512:(g + 1) * 512], w=[('yTg', b)])

    def emit_mm(tt):
        g, j = tt // 4, tt % 4
        b = g % 2
        i3 = tt % NB3
        if j == 0:
            emit_load(g)
        kb.dma('sp', xin[i3][:], xsrc[tt * 128:(tt + 1) * 128, :], w=[('fx', i3)])
        for nh in range(2):
            for k in range(8):
                kb.op('pe', lambda e, nh=nh, k=k: e.matmul(
                    yps[i3][:, nh * 512:(nh + 1) * 512], yTg[b][:, k, j * 128:(j + 1) * 128], wout[:, k, nh * 512:(nh + 1) * 512],
                    start=(k == 0), stop=(k == 7)),
                    r=[('yTg', b)] + wkeys, w=[('yps', i3)])

    def emit_mid(tt):
        i2 = tt % 2
        i3 = tt % NB3
        i4 = tt % 4
        for nh in range(2):
            kb.op('act', lambda e, nh=nh: e.activation(
                out=junk[:, nh * 512:(nh + 1) * 512], in_=yps[i3][:, nh * 512:(nh + 1) * 512], func=AF.Square,
                accum_out=ss[i4][:, nh:nh + 1]),
                r=[('yps', i3)], w=['fjunk', ('fss', i4)])
        kb.op('dve', lambda e: e.tensor_tensor(out=ss[i4][:, 2:3], in0=ss[i4][:, 0:1], in1=ss[i4][:, 1:2], op=ALU.add),
              r=[('fss', i4)], w=[('fss', i4)])
        kb.op('act', lambda e: e.activation(out=rs[i4][:, 0:1], in_=ss[i4][:, 2:3], func=AF.Sqrt, bias=C.cst[:, 0:1], scale=1.0 / D),
              r=[('fss', i4), 'cst'], w=[('frs', i4)])
        kb.op('dve', lambda e: e.reciprocal(rs[i4][:, 0:1], rs[i4][:, 0:1]), r=[('frs', i4)], w=[('frs', i4)])
        kb.op('dve', lambda e: e.scalar_tensor_tensor(
            out=tmp[i2][:], in0=yps[i3][:], scalar=rs[i4][:, 0:1], in1=G1, op0=ALU.mult, op1=ALU.mult),
            r=[('yps', i3), ('frs', i4), ('modt', 2)], w=[('ft', i2)])
        kb.op('pool', lambda e: e.tensor_tensor(out=xn[i2][:], in0=tmp[i2][:], in1=xin[i3][:], op=ALU.add),
              r=[('ft', i2), ('fx', i3)], w=[('fxn', i2)])
        kb.dma('sp', C.xmid[tt * 128:(tt + 1) * 128, :], xn[i2][:], r=[('fxn', i2)])
        kb.op('act', lambda e: e.activation(out=junk[:], in_=xn[i2][:], func=AF.Square, accum_out=ss[i4][:, 3:4]),
              r=[('fxn', i2)], w=['fjunk', ('fss2', i4)])
        kb.op('act', lambda e: e.activation(out=rs[i4][:, 1:2], in_=ss[i4][:, 3:4], func=AF.Sqrt, bias=C.cst[:, 0:1], scale=1.0 / D),
              r=[('fss2', i4), 'cst'], w=[('frs2', i4)])
        kb.op('dve', lambda e: e.reciprocal(rs[i4][:, 1:2], rs[i4][:, 1:2]), r=[('frs2', i4)], w=[('frs2', i4)])
        kb.op('dve', lambda e: e.scalar_tensor_tensor(
            out=tmp[i2][:], in0=xn[i2][:], scalar=rs[i4][:, 1:2], in1=A2, op0=ALU.mult, op1=ALU.mult),
            r=[('fxn', i2), ('frs2', i4), ('modt', 3)], w=[('ft', i2)])
        kb.op('pool', lambda e: e.tensor_tensor(out=h2[i2][:], in0=tmp[i2][:], in1=B2, op=ALU.add),
              r=[('ft', i2), ('modt', 4)], w=[('fh', i2)])
        kb.op('act', lambda e: e.activation(out=h2b[i2][:], in_=h2[i2][:], func=AF.Copy),
              r=[('fh', i2)], w=[('fhb', i2)])

    def emit_tr(tt):
        g, j = tt // 4, tt % 4
        b = g % 2
        i2 = tt % 2
        for k in range(8):
            kb.op('pe', lambda e, k=k: e.transpose(pt[i2][:, k, :], h2b[i2][:, k * 128:(k + 1) * 128], C.identb[:]),
                  r=[('fhb', i2), 'identb'], w=[('fpt', i2)])
        kb.op('act', lambda e: e.activation(out=h2Tg[b][:, :, j * 128:(j + 1) * 128], in_=pt[i2][:], func=AF.Copy),
              r=[('fpt', i2)], w=[('h2Tg', b)])
        if moe:
            for e_ in range(8):
                kb.op('dve', lambda e, e_=e_: e.scalar_tensor_tensor(
                    out=rjunk[:], in0=h2[i2][:], scalar=1.0, in1=rwb[:, e_, :], op0=ALU.mult, op1=ALU.mult,
                    accum_out=lg[i2][:, e_:e_ + 1]),
                    r=[('fh', i2), ('rwb', e_)], w=['rjunk', ('lg', i2)])
            s = sm[i2]
            kb.op('dve', lambda e: e.reduce_max(out=s[:, 0:1], in_=lg[i2][:], axis=AX.X), r=[('lg', i2)], w=[('sm', i2)])
            kb.op('dve', lambda e: e.tensor_scalar(eq1[i2][:], lg[i2][:], s[:, 0:1], None, ALU.is_equal),
                  r=[('lg', i2), ('sm', i2)], w=[('eq1', i2)])
            kb.op('dve', lambda e: e.scalar_tensor_tensor(out=l2[i2][:], in0=eq1[i2][:], scalar=-1e30, in1=lg[i2][:],
                                                          op0=ALU.mult, op1=ALU.add),
                  r=[('eq1', i2), ('lg', i2)], w=[('l2', i2)])
            kb.op('dve', lambda e: e.reduce_max(out=s[:, 1:2], in_=l2[i2][:], axis=AX.X), r=[('l2', i2)], w=[('sm', i2)])
            kb.op('dve', lambda e: e.tensor_scalar(eq2[i2][:], l2[i2][:], s[:, 1:2], None, ALU.is_equal),
                  r=[('l2', i2), ('sm', i2)], w=[('eq2', i2)])
            kb.op('dve', lambda e: e.tensor_tensor(out=s[:, 2:3], in0=s[:, 1:2], in1=s[:, 0:1], op=ALU.subtract),
                  r=[('sm', i2)], w=[('sm', i2)])
            kb.op('act', lambda e: e.activation(out=s[:, 3:4], in_=s[:, 2:3], func=AF.Exp), r=[('sm', i2)], w=[('sm', i2)])
            kb.op('dve', lambda e: e.tensor_scalar(s[:, 4:5], s[:, 3:4], 1.0, None, ALU.add), r=[('sm', i2)], w=[('sm', i2)])
            kb.op('dve', lambda e: e.reciprocal(s[:, 5:6], s[:, 4:5]), r=[('sm', i2)], w=[('sm', i2)])
            kb.op('dve', lambda e: e.tensor_tensor(out=s[:, 6:7], in0=s[:, 3:4], in1=s[:, 5:6], op=ALU.mult),
                  r=[('sm', i2)], w=[('sm', i2)])
            kb.op('dve', lambda e: e.tensor_scalar(eq1[i2][:], eq1[i2][:], s[:, 5:6], None, ALU.mult),
                  r=[('eq1', i2), ('sm', i2)], w=[('eq1', i2)])
            kb.op('dve', lambda e: e.scalar_tensor_tensor(
                out=C.gates[:, tt, :], in0=eq2[i2][:], scalar=s[:, 6:7], in1=eq1[i2][:], op0=ALU.mult, op1=ALU.add),
                r=[('eq2', i2), ('sm', i2), ('eq1', i2)], w=[('gates', tt)])
        if j == 3:
            kb.dma('sp', h2v[:, :, g * 512:(g + 1) * 512], h2Tg[b][:], r=[('h2Tg', b)])

    LA = 2
    for tt in range(min(LA, NTT)):
        emit_mm(tt)
    for tt in range(NTT):
        emit_mid(tt)
        if tt + LA < NTT:
            emit_mm(tt + LA)
        emit_tr(tt)
    kb.end()


def declare4(C, nc):
    def din(name, shape, dt=F32):
        return nc.dram_tensor(name, list(shape), dt, kind="ExternalInput").ap()
    FF = 3584
    C.ffn_w1 = din("ffn_w1", [D, FF])
    C.ffn_w3 = din("ffn_w3", [D, FF])
    C.ffn_w2 = din("ffn_w2", [FF, D])
    C.moe_w1 = din("moe_w1", [8, D, FF])
    C.moe_w3 = din("moe_w3", [8, D, FF])
    C.moe_w2 = din("moe_w2", [8, FF, D])


def phase_FFN(C, l, moe, tbs=range(4)):
    kb = C.kb
    G2 = C.modt[:, 5, :]
    h2v = C.h2T.rearrange("(k p) t -> p k t", p=128)
    for tb in tbs:
        kb.begin()
        h2t = kb.alloc("h2t", [128, 8, 1024], BF16)
        acc = kb.alloc("acc", [128, 8, D], F32)
        aT = kb.alloc("aT", [128, 14, 1024], BF16)
        w2h = [kb.alloc(f"w2h{i}", [128, 14, D], BF16) for i in range(2)]
        w1b = [kb.alloc(f"w1b{i}", [128, 8, 256], BF16) for i in range(2)]
        w3b = [kb.alloc(f"w3b{i}", [128, 8, 256], BF16) for i in range(2)]
        sg = [kb.alloc(f"sg{i}", [128, 512], BF16) for i in range(2)]
        gp = [kb.palloc(f"gp{i}", [128, 512], F32) for i in range(2)]
        up = [kb.palloc(f"up{i}", [128, 512], F32) for i in range(2)]
        yp = [kb.palloc(f"yp{i}", [128, 512], F32) for i in range(3)]
        for k in range(8):
            kb.dma('sp', h2t[:, k, :], h2v[:, k, tb * 1024:(tb + 1) * 1024], w=[('h2t', k)])
        hkeys = [('h2t', k) for k in range(8)]
        experts = list(range(8)) if moe else [None]
        nblk = 0
        nhalf = 0
        ngu = 0
        nyp = 0
        first = True
        for e_ in experts:
            if moe:
                W1, W3, W2 = C.moe_w1[e_], C.moe_w3[e_], C.moe_w2[e_]
            else:
                W1, W3, W2 = C.ffn_w1, C.ffn_w3, C.ffn_w2
            W1v = W1.rearrange("(k p) n -> p k n", p=128)
            W3v = W3.rearrange("(k p) n -> p k n", p=128)
            W2v = W2.rearrange("(c p) n -> p c n", p=128)
            for fh in range(2):
                hb_ = nhalf % 2
                nhalf += 1
                for blk in range(7):
                    wb = nblk % 2
                    nblk += 1
                    c0 = fh * 1792 + blk * 256
                    kb.dma('pool', w1b[wb][:], W1v[:, :, c0:c0 + 256], w=[('w1b', wb)])
                    kb.dma('pool', w3b[wb][:], W3v[:, :, c0:c0 + 256], w=[('w3b', wb)])
                    if blk == 0:
                        kb.dma('pool', w2h[hb_][:, 0:7, :], W2v[:, fh * 14:fh * 14 + 7, :], w=[('w2h', hb_, 0)])
                    if blk == 1:
                        kb.dma('pool', w2h[hb_][:, 7:14, :], W2v[:, fh * 14 + 7:fh * 14 + 14, :], w=[('w2h', hb_, 1)])
                    for fcl in range(2):
                        fc = blk * 2 + fcl
                        for th in range(2):
                            q = ngu % 2
                            ngu += 1
                            for k in range(8):
                                kb.op('pe', lambda e, q=q, wb=wb, k=k, fcl=fcl, th=th: e.matmul(
                                    gp[q][:], w1b[wb][:, k, fcl * 128:(fcl + 1) * 128], h2t[:, k, th * 512:(th + 1) * 512],
                                    start=(k == 0), stop=(k == 7)), r=[('w1b', wb)] + hkeys, w=[('gp', q)])
                            for k in range(8):
                                kb.op('pe', lambda e, q=q, wb=wb, k=k, fcl=fcl, th=th: e.matmul(
                                    up[q][:], w3b[wb][:, k, fcl * 128:(fcl + 1) * 128], h2t[:, k, th * 512:(th + 1) * 512],
                                    start=(k == 0), stop=(k == 7)), r=[('w3b', wb)] + hkeys, w=[('up', q)])
                            kb.op('act', lambda e, q=q: e.activation(out=sg[q][:], in_=gp[q][:], func=AF.Silu),
                                  r=[('gp', q)], w=[('sg', q)])
                            kb.op('dve', lambda e, q=q, fc=fc, th=th: e.tensor_tensor(
                                out=aT[:, fc, th * 512:(th + 1) * 512], in0=sg[q][:], in1=up[q][:], op=ALU.mult),
                                r=[('sg', q), ('up', q)], w=[('aT', fc, th)])
                akeys = [('aT', fc, th) for fc in range(14) for th in range(2)]
                for j in range(8):
                    tt = tb * 8 + j
                    for nh in range(2):
                        y = nyp % 3
                        nyp += 1
                        for fc in range(14):
                            kb.op('pe', lambda e, y=y, fc=fc, j=j, nh=nh, hb_=hb_: e.matmul(
                                yp[y][:], aT[:, fc, j * 128:(j + 1) * 128], w2h[hb_][:, fc, nh * 512:(nh + 1) * 512],
                                start=(fc == 0), stop=(fc == 13)),
                                r=[('aT', fc, j // 4), ('w2h', hb_, fc // 7)], w=[('yp', y)])
                        dst = acc[:, j, nh * 512:(nh + 1) * 512]
                        akey = ('acc', j, nh)
                        if first:
                            if moe:
                                kb.op('dve', lambda e, dst=dst, y=y, tt=tt, e_=e_: e.tensor_scalar(
                                    dst, yp[y][:], C.gates[:, tt, e_:e_ + 1], None, ALU.mult),
                                    r=[('yp', y), ('gates', tt)], w=[akey])
                            else:
                                kb.op('act', lambda e, dst=dst, y=y: e.activation(out=dst, in_=yp[y][:], func=AF.Copy),
                                      r=[('yp', y)], w=[akey])
                        else:
                            if moe:
                                kb.op('dve', lambda e, dst=dst, y=y, tt=tt, e_=e_: e.scalar_tensor_tensor(
                                    out=dst, in0=yp[y][:], scalar=C.gates[:, tt, e_:e_ + 1], in1=dst, op0=ALU.mult, op1=ALU.add),
                                    r=[('yp', y), ('gates', tt), akey], w=[akey])
                            else:
                                kb.op('dve', lambda e, dst=dst, y=y: e.tensor_tensor(out=dst, in0=yp[y][:], in1=dst, op=ALU.add),
                                      r=[('yp', y), akey], w=[akey])
                first = False
        xm = [kb.alloc(f"xm{i}", [128, D], F32) for i in range(2)]
        t2 = [kb.alloc(f"t2{i}", [128, D], F32) for i in range(2)]
        junk = kb.alloc("gjunk", [128, D], BF16)
        ss = kb.alloc("gss", [128, 8], F32)
        rs = kb.alloc("grs", [128, 8], F32)
        for j in range(8):
            tt = tb * 8 + j
            i2 = j % 2
            kb.dma('sp', xm[i2][:], C.xmid[tt * 128:(tt + 1) * 128, :], w=[('xm', i2)])
            kb.op('act', lambda e, j=j: e.activation(out=junk[:], in_=acc[:, j, :], func=AF.Square, accum_out=ss[:, j:j + 1]),
                  r=[('acc', j, 0), ('acc', j, 1)], w=['gjunk', ('gss', j)])
            kb.op('act', lambda e, j=j: e.activation(out=rs[:, j:j + 1], in_=ss[:, j:j + 1], func=AF.Sqrt, bias=C.cst[:, 0:1], scale=1.0 / D),
                  r=[('gss', j), 'cst'], w=[('grs', j)])
            kb.op('dve', lambda e, j=j: e.reciprocal(rs[:, j:j + 1], rs[:, j:j + 1]), r=[('grs', j)], w=[('grs', j)])
            kb.op('dve', lambda e, j=j, i2=i2: e.scalar_tensor_tensor(
                out=t2[i2][:], in0=acc[:, j, :], scalar=rs[:, j:j + 1], in1=G2, op0=ALU.mult, op1=ALU.mult),
                r=[('acc', j, 0), ('acc', j, 1), ('grs', j), ('modt', 5)], w=[('t2', i2)])
            kb.op('pool', lambda e, i2=i2: e.tensor_tensor(out=t2[i2][:], in0=t2[i2][:], in1=xm[i2][:], op=ALU.add),
                  r=[('t2', i2), ('xm', i2)], w=[('t2', i2)])
            kb.dma('sp', C.out[tt * 128:(tt + 1) * 128, :], t2[i2][:], r=[('t2', i2)])
        kb.end()


def host_na_table(rpb):
    tab = np.full((14, 4, 128, 256), -30000.0, np.float32)
    qc = np.arange(64)
    cs = np.clip(qc - 8, 0, 48)
    kinds = [('g', m) for m in range(6)] + [('e0', m) for m in range(4)] + [('e15', m) for m in range(4)]
    for ti, (kind, m) in enumerate(kinds):
        for a in range(2):
            for i in range(4):
                dr = (2 * m + a - i) if kind == 'e0' else (2 * m - 4 + a - i)
                if kind == 'g' and not (-4 <= dr <= 3):
                    continue
                for kc in range(64):
                    v = (kc >= cs) & (kc < cs + 16)
                    tab[ti, :, a * 64 + kc, i * 64 + qc[v]] = rpb[:, dr + 7, kc - qc[v] + 15].T
    return np.ascontiguousarray(tab.transpose(2, 0, 1, 3))


def declare5(C, nc):
    C.na_tab = nc.dram_tensor("na_tab", [2, 128, 14, 4, 256], F32, kind="ExternalInput").ap()


def phase_NA(C, l):
    import os
    STG = int(os.environ.get("NA_STAGE", "9"))
    kb = C.kb
    kb.begin()
    q2 = [kb.alloc(f"q2{i}", [128, T], BF16) for i in range(2)]
    k2 = [kb.alloc(f"k2{i}", [128, T], BF16) for i in range(2)]
    vt = kb.alloc("vt", [128, NTT, G], BF16)
    btab = kb.alloc("btab", [128, 14, 4, 256], BF16)
    yall = kb.alloc("yall", [64, 4, T], BF16)
    sb = [kb.alloc(f"sb{i}", [128, 2, 256], F32) for i in range(4)]
    pT = [kb.alloc(f"pT{i}", [128, 2, 256], BF16) for i in range(4)]
    rden = [kb.alloc(f"rden{i}", [64, 2, 256], F32) for i in range(2)]
    sp = [kb.palloc(f"sp{i}", [128, 2, 256], F32) for i in range(4)]
    ops_ = [kb.palloc(f"ops{i}", [64, 512], F32) for i in range(2)]
    dps = [kb.palloc(f"dps{i}", [64, 512], F32) for i in range(2)]
    for hp in range(2):
        kb.dma('sp', q2[hp][:], C.zT_bf[hp * 128:(hp + 1) * 128, :], w=[('q2', hp)])
        kb.dma('sp', k2[hp][:], C.zT_bf[256 + hp * 128:256 + (hp + 1) * 128, :], w=[('k2', hp)])
    kb.dma('sp', vt[:], C.vtok.rearrange("(n p) c -> p n c", p=128), w=['vt'])
    for ti in range(14):
        kb.dma('pool', btab[:, ti], C.na_tab[l, :, ti], w=['btab'])
    kz = [[kb.alloc(f"kz{hp}{h2}", [128, T], BF16) for h2 in range(2)] for hp in range(2)]
    for hp in range(2):
        for h2 in range(2):
            oth = slice((1 - h2) * 64, (2 - h2) * 64)
            me = slice(h2 * 64, (h2 + 1) * 64)
            kb.op('pool', lambda e, hp=hp, h2=h2, oth=oth: e.memset(kz[hp][h2][oth, :], 0.0), w=[('kz', hp, h2)])
            kb.op('dve', lambda e, hp=hp, h2=h2, me=me: e.tensor_copy(out=kz[hp][h2][me, :], in_=k2[hp][me, :]),
                  r=[('k2', hp)], w=[('kz', hp, h2)])
    items = []
    for qg in range(16):
        if qg == 0:
            tiles = [(m, 6 + m) for m in range(4)]
        elif qg == 15:
            tiles = [(28 + m, 10 + m) for m in range(4)]
        else:
            tiles = [(2 * qg - 2 + m, m) for m in range(6)]
        for hp in range(2):
            for idx, (kt, ti) in enumerate(tiles):
                items.append((qg, hp, idx, kt, ti, len(tiles)))

    def emit_score(j):
        qg, hp, idx, kt, ti, nt = items[j]
        s = j % 4
        qs = slice(qg * 256, (qg + 1) * 256)
        for h2 in range(2):
            kb.op('pe', lambda e, h2=h2: e.matmul(sp[s][:, h2, :], kz[hp][h2][:, kt * 128:(kt + 1) * 128], q2[hp][:, qs], start=True, stop=True),
                  r=[('kz', hp, h2), ('q2', hp)], w=[('sp', s)])

    def emit_post(j):
        qg, hp, idx, kt, ti, nt = items[j]
        s = j % 4
        kb.op('dve', lambda e: e.scalar_tensor_tensor(
            out=sb[s][:], in0=sp[s][:], scalar=0.125, in1=btab[:, ti, 2 * hp:2 * hp + 2, :], op0=ALU.mult, op1=ALU.add),
            r=[('sp', s), 'btab'], w=[('sb', s)])
        kb.op('act', lambda e: e.activation(out=pT[s][:], in_=sb[s][:], func=AF.Exp), r=[('sb', s)], w=[('pT', s)])

    def emit_pv(j):
        qg, hp, idx, kt, ti, nt = items[j]
        s = j % 4
        qs = slice(qg * 256, (qg + 1) * 256)
        for h2 in range(2):
            h = 2 * hp + h2
            kb.op('pe', lambda e, h2=h2, h=h: e.matmul(
                ops_[h2][:, 0:256], vt[:, kt, h * 64:(h + 1) * 64], pT[s][:, h2, :], start=(idx == 0), stop=(idx == nt - 1)),
                r=['vt', ('pT', s)], w=[('ops', h2)])
            kb.op('pe', lambda e, h2=h2: e.matmul(
                dps[h2][:, 0:256], C.ones_b[:, 0:64], pT[s][:, h2, :], start=(idx == 0), stop=(idx == nt - 1)),
                r=['ones_b', ('pT', s)], w=[('dps', h2)])
        if idx == nt - 1:
            for h2 in range(2):
                kb.op('dve', lambda e, h2=h2: e.reciprocal(rden[0][:, h2, :], dps[h2][:, 0:256]), r=[('dps', h2)], w=[('rden', h2)])
                kb.op('dve', lambda e, h2=h2: e.tensor_tensor(
                    out=yall[:, 2 * hp + h2, qs], in0=ops_[h2][:, 0:256], in1=rden[0][:, h2, :], op=ALU.mult),
                    r=[('ops', h2), ('rden', h2)], w=[('yall', hp)])

    NI = len(items)
    LA = 2
    for j in range(min(LA, NI)):
        emit_score(j)
    for j in range(NI):
        emit_post(j)
        if j + LA < NI:
            emit_score(j + LA)
        emit_pv(j)
    for h in range(4):
        kb.dma('sp', C.yT[h * 64:(h + 1) * 64, :], yall[:, h, :], r=[('yall', h // 2)])
    kb.end()


TBK = 1024
NCH = 32
E05 = 0.6065306597126334


def host_rw_masks():
    idx = np.arange(128)
    s = idx[:, None]
    t = idx[None, :]
    m = np.zeros((2, 128, 256), np.float32)
    m[0, :, 0:128] = (t > s)
    m[0, :, 128:256] = (t >= s)
    m[1, :, 0:128] = (t < s)
    m[1, :, 128:256] = (t <= s)
    return m


def host_blk():
    b = np.zeros((128, 128), np.float32)
    b[0:64, 0:64] = 1.0
    b[64:128, 64:128] = 1.0
    return b


def declare6(C, nc, kinds=None):
    kinds = kinds or {}

    def din(name, shape, dt=F32):
        return nc.dram_tensor(name, list(shape), dt, kind="ExternalInput").ap()

    def scr(name, shape, dt):
        return nc.dram_tensor(name, list(shape), dt, kind=kinds.get(name, "Internal")).ap()
    C.rw_w2 = din("rw_w2", [2, 2, 32, G])
    C.rw_a2 = din("rw_a2", [2, 2, 32, G])
    C.rw_g2 = din("rw_g2", [2, 64, G])
    C.rw_masks = din("rw_masks", [2, 128, 256])
    C.blk = din("blk", [128, 128])
    C.prep = scr("rw_prep", [2, 5, G, T], BF16)
    C.egc = scr("rw_egc", [2, G, NCH], F32)
    C.Yd = scr("rw_Y", [2, T, G], F32)


def phase_R1(C, l, e):
    kb = C.kb
    kb.begin()
    fwd = (e == 0)
    TB = 512
    NB = T // TB
    NC_ = TB // 128
    ppt = kb.alloc("ppt", [128, NPP], F32)
    kb.dma('sp', ppt[:], C.pp[l], w=['ppt'])
    omu = kb.alloc("omu", [128, 7], F32)
    omka = kb.alloc("omka", [128, 2], F32)
    mu0 = PP[f'mu{e}']
    kb.op('dve', lambda e_: e_.tensor_scalar(omu[:], ppt[:, mu0:mu0 + 7], -1.0, 1.0, ALU.mult, ALU.add), r=['ppt'], w=['omu'])
    kb.op('dve', lambda e_: e_.tensor_scalar(omka[:], ppt[:, PP['k_a']:PP['k_a'] + 2], -1.0, 1.0, ALU.mult, ALU.add), r=['ppt'], w=['omka'])
    w2z = kb.alloc("w2z", [64, G], F32)
    a2z = kb.alloc("a2z", [64, G], F32)
    blk = kb.alloc("blkf", [128, 128], F32)
    kb.op('dve', lambda e_: e_.memset(w2z[:], 0.0), w=['w2z'])
    kb.op('dve', lambda e_: e_.memset(a2z[:], 0.0), w=['a2z'])
    kb.dma('sp', w2z[0:32, :], C.rw_w2[l, e], w=['w2z'])
    kb.dma('sp', a2z[32:64, :], C.rw_a2[l, e], w=['a2z'])
    kb.dma('sp', blk[:], C.blk[:, :], w=['blkf'])
    egc = kb.alloc("egc", [128, 2, NCH], F32)
    PW = 192

    class Buf:
        def __init__(self, name, shape, dt, n=2, zero=False):
            self.t = [kb.alloc(f"{name}{i}", shape, dt) for i in range(n)]
            self.name = name
            self.n = n
            if zero:
                for i, t_ in enumerate(self.t):
                    kb.op('dve', lambda e_, t_=t_: e_.memset(t_[:], 0.0), w=[(name, i)])

        def get(self, it):
            return self.t[it % self.n], (self.name, it % self.n)

    Zx = {n: Buf(f"Zx{n}", [128, TB + 1], F32, n=3) for n in ('r', 'k', 'v', 'l')}
    tmpB = Buf("r1tmp", [128, TB], F32, n=3)
    zdB = {n: Buf(f"zd{n}", [128, TB], F32, n=3) for n in ('r', 'k', 'v', 'l')}
    tlB = Buf("tl", [64, TB], F32, n=3)
    sigB = Buf("sig", [128, TB], F32, n=3)
    asgB = Buf("asg", [128, TB], F32, n=3)
    scAB = Buf("scA", [128, NC_, PW], F32, n=3, zero=True)
    scBB = Buf("scB", [128, NC_, PW], F32, n=3, zero=True)
    E1B = Buf("E1", [128, TB], F32)
    E2B = Buf("E2", [128, TB], F32)
    E3B = Buf("E3", [128, TB], F32)
    kkB = Buf("kk", [128, TB], F32, n=3)
    kk2B = Buf("kk2", [128, TB], F32, n=3)
    rnB = Buf("rn", [128, TB], F32, n=3)
    facB = Buf("fac", [128, TB], F32)
    ob = {n: Buf(f"ob{n}", [128, TB], BF16) for n in ('a', 'r', 'k', 'b', 'v')}
    psw = [kb.palloc(f"psw{i}", [128, 512], F32) for i in range(2)]
    psa = [kb.palloc(f"psa{i}", [128, 512], F32) for i in range(2)]
    pss = [kb.palloc(f"pss{i}", [128, 512], F32) for i in range(2)]
    if fwd:
        cen = slice(1, TB + 1)
        sh = slice(0, TB)
        dat = slice(64, 192)
    else:
        cen = slice(0, TB)
        sh = slice(1, TB + 1)
        dat = slice(0, 128)
    nmix_box = [0]

    def load(name, row0, itn, tb):
        t0 = tb * TB
        z, key = Zx[name].get(itn)
        rows = C.zT_rw[row0:row0 + 128, :]
        if fwd:
            if tb == 0:
                kb.op('dve', lambda e_, z=z: e_.memset(z[:, 0:1], 0.0), w=[key])
                kb.dma('sp', z[:, 1:TB + 1], rows[:, 0:TB], w=[key])
            else:
                kb.dma('sp', z[:, :], rows[:, t0 - 1:t0 + TB], w=[key])
        else:
            if tb == NB - 1:
                kb.op('dve', lambda e_, z=z: e_.memset(z[:, TB:TB + 1], 0.0), w=[key])
                kb.dma('sp', z[:, 0:TB], rows[:, t0:T], w=[key])
            else:
                kb.dma('sp', z[:, :], rows[:, t0:t0 + TB + 1], w=[key])
        return z, key

    def mix(z, zkey, out, okey, mucol):
        tmp, tkey = tmpB.get(nmix_box[0])
        nmix_box[0] += 1
        kb.op('act', lambda e_: e_.activation(out=tmp[:], in_=z[:, cen], func=AF.Copy, scale=omu[:, mucol:mucol + 1]),
              r=[zkey, 'omu'], w=[tkey])
        kb.op('dve', lambda e_: e_.scalar_tensor_tensor(out=out[:], in0=z[:, sh], scalar=ppt[:, mu0 + mucol:mu0 + mucol + 1],
                                                        in1=tmp[:], op0=ALU.mult, op1=ALU.add),
              r=[zkey, 'ppt', tkey], w=[okey])

    def stage_X(it):
        tb, hp = it // 2, it % 2
        if hp == 0:
            z, zk = load('l', 768, tb, tb)
            zdl, zdlk = zdB['l'].get(tb)
            mix(z, zk, zdl, zdlk, 6)
            tl, tlk = tlB.get(tb)
            kb.op('act', lambda e_: e_.activation(out=tl[0:32, :], in_=zdl[0:32, :], func=AF.Tanh), r=[zdlk], w=[tlk])
            kb.op('act', lambda e_: e_.activation(out=tl[32:64, :], in_=zdl[32:64, :], func=AF.Copy), r=[zdlk], w=[tlk])
        tl, tlk = tlB.get(tb)
        chs = slice(hp * 128, (hp + 1) * 128)
        zr, zrk = load('r', hp * 128, it, tb)
        zk_, zkk = load('k', 256 + hp * 128, it, tb)
        zv, zvk = load('v', 512 + hp * 128, it, tb)
        zdr, zdrk = zdB['r'].get(it)
        zdk, zdkk = zdB['k'].get(it)
        zdv, zdvk = zdB['v'].get(it)
        mix(zr, zrk, zdr, zdrk, hp)
        mix(zk_, zkk, zdk, zdkk, 2 + hp)
        mix(zv, zvk, zdv, zdvk, 4 + hp)
        sig, sigk = sigB.get(it)
        asg, asgk = asgB.get(it)
        q = it % 2
        kb.op('pe', lambda e_: e_.matmul(psw[q][:], w2z[:, chs], tl[:], start=True, stop=True), r=['w2z', tlk], w=[('psw', q)])
        kb.op('pe', lambda e_: e_.matmul(psa[q][:], a2z[:, chs], tl[:], start=True, stop=True), r=['a2z', tlk], w=[('psa', q)])
        w0c = PP[f'w0_{e}'] + hp
        a0c = PP[f'a0_{e}'] + hp
        kb.op('act', lambda e_: e_.activation(out=sig[:], in_=psw[q][:], func=AF.Sigmoid, bias=ppt[:, w0c:w0c + 1]),
              r=[('psw', q), 'ppt'], w=[sigk])
        kb.op('act', lambda e_: e_.activation(out=asg[:], in_=psa[q][:], func=AF.Sigmoid, bias=ppt[:, a0c:a0c + 1]),
              r=[('psa', q), 'ppt'], w=[asgk])
        scA, scAk = scAB.get(it)
        kb.op('act', lambda e_: e_.activation(out=scA[:, :, dat], in_=sig[:].rearrange("p (c t) -> p c t", t=128), func=AF.Copy, scale=-E05),
              r=[sigk], w=[scAk])
        kkc = PP['k_k'] + hp
        kk, kkk = kkB.get(it)
        kk2, kk2k = kk2B.get(it)
        rn, rnk = rnB.get(it)
        kb.op('dve', lambda e_: e_.tensor_scalar(kk[:], zdk[:], ppt[:, kkc:kkc + 1], None, ALU.mult), r=[zdkk, 'ppt'], w=[kkk])
        kb.op('act', lambda e_: e_.activation(out=kk2[:], in_=zdk[:], func=AF.Square, scale=ppt[:, kkc:kkc + 1]), r=[zdkk, 'ppt'], w=[kk2k])
        kb.op('pe', lambda e_: e_.matmul(pss[q][:], blk[:], kk2[:], start=True, stop=True), r=['blkf', kk2k], w=[('pss', q)])
        kb.op('dve', lambda e_: e_.tensor_scalar(rn[:], pss[q][:], 1e-12, None, ALU.max), r=[('pss', q)], w=[rnk])

    def stage_Y(it):
        tb, hp = it // 2, it % 2
        t0 = tb * TB
        zdr, zdrk = zdB['r'].get(it)
        zdk, zdkk = zdB['k'].get(it)
        zdv, zdvk = zdB['v'].get(it)
        asg, asgk = asgB.get(it)
        scA, scAk = scAB.get(it)
        scB, scBk = scBB.get(it)
        kk, kkk = kkB.get(it)
        kk2, kk2k = kk2B.get(it)
        rn, rnk = rnB.get(it)
        src, dst, sk, dk = scA, scB, scAk, scBk
        for d in (1, 2, 4, 8, 16, 32, 64):
            if fwd:
                shd = slice(64 - d, 192 - d)
            else:
                shd = slice(d, 128 + d)
            kb.op('dve', lambda e_, src=src, dst=dst, shd=shd: e_.tensor_tensor(out=dst[:, :, dat], in0=src[:, :, dat], in1=src[:, :, shd],
                                                                               op=ALU.add),
                  r=[sk], w=[dk])
            yield
            src, dst, sk, dk = dst, src, dk, sk
        ci, cik = src, sk
        if fwd:
            cex = slice(63, 191)
            gcol = 191
        else:
            cex = slice(1, 129)
            gcol = 0
        v3 = lambda tle: tle[:].rearrange("p (c t) -> p c t", t=128)
        E1, E1k = E1B.get(it)
        E2, E2k = E2B.get(it)
        E3, E3k = E3B.get(it)
        fac, fack = facB.get(it)
        kb.op('act', lambda e_: e_.activation(out=v3(E1), in_=ci[:, :, dat], func=AF.Exp), r=[cik], w=[E1k])
        yield
        kb.op('act', lambda e_: e_.activation(out=v3(E2), in_=ci[:, :, dat], func=AF.Exp, scale=-1.0), r=[cik], w=[E2k])
        yield
        kb.op('act', lambda e_: e_.activation(out=v3(E3), in_=ci[:, :, cex], func=AF.Exp), r=[cik], w=[E3k])
        yield
        kb.op('act', lambda e_: e_.activation(out=egc[:, hp, tb * NC_:(tb + 1) * NC_], in_=ci[:, :, gcol], func=AF.Exp),
              r=[cik], w=[('egc', hp)])
        yield
        kb.op('act', lambda e_: e_.activation(out=rn[:], in_=rn[:], func=AF.Sqrt), r=[rnk], w=[rnk])
        yield
        kb.op('dve', lambda e_: e_.reciprocal(rn[:], rn[:]), r=[rnk], w=[rnk])
        yield
        kb.op('dve', lambda e_: e_.tensor_tensor(out=kk[:], in0=kk[:], in1=rn[:], op=ALU.mult), r=[kkk, rnk], w=[kkk])
        yield
        kac = PP['k_a'] + hp
        kb.op('act', lambda e_: e_.activation(out=fac[:], in_=asg[:], func=AF.Identity, scale=ppt[:, kac:kac + 1], bias=omka[:, hp:hp + 1]),
              r=[asgk, 'ppt', 'omka'], w=[fack])
        yield
        kb.op('dve', lambda e_: e_.tensor_tensor(out=fac[:], in0=fac[:], in1=zdk[:], op=ALU.mult), r=[fack, zdkk], w=[fack])
        yield
        oa, oak = ob['a'].get(it)
        ok_, okk = ob['k'].get(it)
        ob_, obk = ob['b'].get(it)
        or_, ork = ob['r'].get(it)
        ov, ovk = ob['v'].get(it)
        kb.op('dve', lambda e_: e_.scalar_tensor_tensor(out=oa[:], in0=kk[:], scalar=-1.0, in1=E3[:], op0=ALU.mult, op1=ALU.mult),
              r=[kkk, E3k], w=[oak])
        yield
        kb.op('pool', lambda e_: e_.tensor_tensor(out=ok_[:], in0=fac[:], in1=E2[:], op=ALU.mult), r=[fack, E2k], w=[okk])
        yield
        kb.op('dve', lambda e_: e_.tensor_tensor(out=kk2[:], in0=kk[:], in1=asg[:], op=ALU.mult), r=[kkk, asgk], w=[kk2k])
        yield
        kb.op('dve', lambda e_: e_.tensor_tensor(out=ob_[:], in0=kk2[:], in1=E2[:], op=ALU.mult), r=[kk2k, E2k], w=[obk])
        yield
        kb.op('pool', lambda e_: e_.tensor_tensor(out=or_[:], in0=zdr[:], in1=E1[:], op=ALU.mult), r=[zdrk, E1k], w=[ork])
        yield
        kb.op('act', lambda e_: e_.activation(out=ov[:], in_=zdv[:], func=AF.Copy), r=[zdvk], w=[ovk])
        yield
        for qi, (tile_, key_) in enumerate(((oa, oak), (or_, ork), (ok_, okk), (ob_, obk), (ov, ovk))):
            kb.dma('sp', C.prep[e, qi, hp * 128:(hp + 1) * 128, t0:t0 + TB], tile_[:], r=[key_])
            yield

    NIT = NB * 2
    LA = 2
    for it in range(min(LA, NIT)):
        stage_X(it)
    for it in range(0, NIT, 2):
        gens = [stage_Y(it), stage_Y(it + 1)]
        while gens:
            for g_ in list(gens):
                try:
                    next(g_)
                except StopIteration:
                    gens.remove(g_)
        for it2 in (it + LA, it + LA + 1):
            if it2 < NIT:
                stage_X(it2)
    for hp in range(2):
        kb.dma('sp', C.egc[e, hp * 128:(hp + 1) * 128, :], egc[:, hp, :], r=[('egc', hp)])
    kb.end()


def phase_R2(C, l, e, nchunks=NCH):
    kb = C.kb
    kb.begin()
    fwd = (e == 0)
    names = ('a', 'r', 'k', 'b', 'v')
    pre = {}
    for qi, n_ in enumerate(names):
        pre[n_] = kb.alloc(f"pre_{n_}", [128, 2, T], BF16)
        for hp in range(2):
            kb.dma('sp', pre[n_][:, hp, :], C.prep[e, qi, hp * 128:(hp + 1) * 128, :], w=[('pre', n_)])
    egc = kb.alloc("egc2", [128, 2, NCH], F32)
    for hp in range(2):
        kb.dma('sp', egc[:, hp, :], C.egc[e, hp * 128:(hp + 1) * 128, :], w=['egc2'])
    mask = kb.alloc("maskLQ", [128, 256], BF16)
    kb.dma('pool', mask[:], C.rw_masks[e], w=['mask'])
    maskN = kb.alloc("maskN", [128, 128], BF16)
    kb.dma('pool', maskN[:], C.rw_masks[1 - e, :, 0:128], w=['maskN'])
    kzb = [kb.alloc(f"kzb{i}", [128, 4, 128], BF16) for i in range(2)]
    bzb = [kb.alloc(f"bzb{i}", [128, 4, 128], BF16) for i in range(2)]
    azb = [kb.alloc(f"azb{i}", [128, 4, 128], BF16) for i in range(2)]
    for i in range(2):
        kb.op('dve', lambda e_, i=i: e_.memset(kzb[i][:], 0.0), w=[('kzb', i)])
        kb.op('dve', lambda e_, i=i: e_.memset(bzb[i][:], 0.0), w=[('bzb', i)])
        kb.op('dve', lambda e_, i=i: e_.memset(azb[i][:], 0.0), w=[('azb', i)])
    khat = [kb.alloc(f"khat{i}", [128, 2, 128], BF16) for i in range(2)]
    bhat = [kb.alloc(f"bhat{i}", [128, 2, 128], BF16) for i in range(2)]
    DG = [kb.alloc(f"DG{i}", [128, 2, 128], BF16) for i in range(2)]
    tokS = kb.alloc("tokS", [128, 2, 4, 128], BF16)
    LQA = kb.alloc("LQA", [128, 4, 256], BF16)
    LQB = kb.alloc("LQB", [128, 4, 256], BF16)
    PPb = [kb.alloc(f"PPb{i}", [128, 4, 2, 128], BF16) for i in range(3)]
    TT = [kb.alloc(f"TT{i}", [128, 4, 128], BF16) for i in range(2)]
    Wsb = kb.alloc("Wsb", [128, 4, 64], BF16)
    AV = kb.alloc("AV", [128, 4, 128], BF16)
    RT = kb.alloc("RT", [64, 4, 128], BF16)
    MT = kb.alloc("MT", [64, 4, 64], BF16)
    H = [kb.alloc(f"H{i}", [64, 4, 64], BF16) for i in range(2)]
    yst = [kb.alloc(f"ryst{i}", [128, 4, 64], F32) for i in range(2)]
    kb.op('dve', lambda e_: e_.memset(H[0][:], 0.0), w=[('H', 0)])
    pb = {i: kb.palloc(f"pb{i}", [128, 512], F32) for i in (0, 1, 2, 3, 6, 7)}
    tokT_t = kb.palloc("tokT", [128, 2, 4, 128], BF16)
    psW_t = kb.palloc("psW", [128, 4, 64], F32)
    identb = C.identb
    idb4 = identb[:, :].unsqueeze(1).to_broadcast([128, 4, 128])
    idb2 = identb[:, :].unsqueeze(1).to_broadcast([128, 2, 128])

    tokT = tokT_t[:]
    psA = [pb[hp][:, :].rearrange("p (u c) -> p u c", u=2) for hp in range(2)]
    psB = [pb[2 + hp][:, :].rearrange("p (u c) -> p u c", u=2) for hp in range(2)]
    psL = pb[6][:, :].rearrange("p (u c) -> p u c", u=4)
    psW = psW_t[:]
    psP = [pb[hp][:, :].rearrange("p (u k c) -> p u k c", u=2, k=2) for hp in range(2)]
    psTh = [pb[2 + hp][:, 0:256].rearrange("p (u c) -> p u c", u=2) for hp in range(2)]
    psAV = pb[3][:, :].rearrange("p (u c) -> p u c", u=4)
    psR = pb[6][0:64, :].rearrange("p (u c) -> p u c", u=4)
    psY = pb[7][:, 0:256].rearrange("p (u c) -> p u c", u=4)
    psM = psW_t[0:64, :, :]
    psN = pb[7][0:64, 256:512].rearrange("p (u c) -> p u c", u=4)
    K_ = lambda i: ('pb', i)

    def stage_a(n):
        c = n if fwd else NCH - 1 - n
        cs = slice(c * 128, (c + 1) * 128)
        i = n % 2
        for h2 in range(2):
            hs = slice(h2 * 64, (h2 + 1) * 64)
            kb.op('pool', lambda e_, hs=hs, h2=h2: e_.tensor_copy(out=kzb[i][hs, h2::2, :], in_=pre['k'][hs, :, cs]),
                  r=[('pre', 'k')], w=[('kzb', i)])
            kb.op('pool', lambda e_, hs=hs, h2=h2: e_.tensor_copy(out=bzb[i][hs, h2::2, :], in_=pre['b'][hs, :, cs]),
                  r=[('pre', 'b')], w=[('bzb', i)])
            kb.op('pool', lambda e_, hs=hs, h2=h2: e_.tensor_copy(out=azb[i][hs, h2::2, :], in_=pre['a'][hs, :, cs]),
                  r=[('pre', 'a')], w=[('azb', i)])
        gbc = egc[:, :, c:c + 1].to_broadcast([128, 2, 128])
        kb.op('pool', lambda e_: e_.tensor_tensor(out=khat[i][:], in0=pre['k'][:, :, cs], in1=gbc, op=ALU.mult),
              r=[('pre', 'k'), 'egc2'], w=[('khat', i)])
        kb.op('pool', lambda e_: e_.tensor_tensor(out=bhat[i][:], in0=pre['b'][:, :, cs], in1=gbc, op=ALU.mult),
              r=[('pre', 'b'), 'egc2'], w=[('bhat', i)])
        kb.op('pool', lambda e_: e_.tensor_tensor(out=DG[i][:], in0=idb2, in1=gbc, op=ALU.mult),
              r=['identb', 'egc2'], w=[('DG', i)])

    stage_a(0)

    def chunk_body(n, cur):
        c = n if fwd else NCH - 1 - n
        cs = slice(c * 128, (c + 1) * 128)
        i = n % 2
        for hp in range(2):
            srcs = [(pre['a'][:, hp, cs], ('pre', 'a')), (khat[i][:, hp, :], ('khat', i)), (bhat[i][:, hp, :], ('bhat', i)),
                    (pre['v'][:, hp, cs], ('pre', 'v'))]
            for q, (sap, skey) in enumerate(srcs):
                kb.op('pe', lambda e_, hp=hp, q=q, sap=sap: e_.transpose(tokT[:, hp, q, :], sap, identb[:]),
                      r=[skey, 'identb'], w=['tokT'])
        kb.op('act', lambda e_: e_.activation(out=tokS[:], in_=tokT, func=AF.Copy), r=['tokT'], w=['tokS'])
        for u in range(4):
            hp, h2 = u // 2, u % 2
            kb.op('pe', lambda e_, u=u, hp=hp, h2=h2: e_.matmul(psA[hp][:, h2, 0:128], kzb[i][:, u, :], pre['a'][:, hp, cs], start=True, stop=True),
                  r=[('kzb', i), ('pre', 'a')], w=[K_(hp)])
            kb.op('pe', lambda e_, u=u, hp=hp, h2=h2: e_.matmul(psA[hp][:, h2, 128:256], kzb[i][:, u, :], pre['r'][:, hp, cs], start=True, stop=True),
                  r=[('kzb', i), ('pre', 'r')], w=[K_(hp)])
        for u in range(4):
            hp, h2 = u // 2, u % 2
            kb.op('pe', lambda e_, u=u, hp=hp, h2=h2: e_.matmul(psB[hp][:, h2, 0:128], bzb[i][:, u, :], pre['a'][:, hp, cs], start=True, stop=True),
                  r=[('bzb', i), ('pre', 'a')], w=[K_(2 + hp), ('psT', 0), ('psT', 1)])
            kb.op('pe', lambda e_, u=u, hp=hp, h2=h2: e_.matmul(psB[hp][:, h2, 128:256], bzb[i][:, u, :], pre['r'][:, hp, cs], start=True, stop=True),
                  r=[('bzb', i), ('pre', 'r')], w=[K_(2 + hp), ('psT', 0), ('psT', 1)])
        for u in range(4):
            kb.op('pe', lambda e_, u=u: e_.matmul(psL[:, u, :], azb[i][:, u, :], pre['b'][:, u // 2, cs], start=True, stop=True),
                  r=[('azb', i), ('pre', 'b')], w=[K_(6)])
        mbc = mask[:, :].unsqueeze(1).to_broadcast([128, 2, 256])
        for hp in range(2):
            kb.op('dve', lambda e_, hp=hp: e_.tensor_tensor(out=LQB[:, 2 * hp:2 * hp + 2, :], in0=psB[hp], in1=mbc, op=ALU.mult),
                  r=[K_(2 + hp), 'mask'], w=['LQB'])
        kb.op('dve', lambda e_: e_.tensor_tensor(out=PPb[0][:, :, 0, :], in0=psL, in1=maskN[:, :].unsqueeze(1).to_broadcast([128, 4, 128]),
                                                 op=ALU.mult),
              r=[K_(6), 'maskN'], w=[('PPb', 0, 0), ('PPb', 0, 1)])
        kb.op('dve', lambda e_: e_.tensor_tensor(out=TT[0][:], in0=LQB[:, :, 0:128], in1=idb4, op=ALU.add),
              r=['LQB', 'identb'], w=[('TT', 0, 0), ('TT', 0, 1)])
        for hp in range(2):
            kb.op('dve', lambda e_, hp=hp: e_.tensor_tensor(out=LQA[:, 2 * hp:2 * hp + 2, :], in0=psA[hp], in1=mbc, op=ALU.mult),
                  r=[K_(hp), 'mask'], w=['LQA'])
        if n + 1 < nchunks:
            stage_a(n + 1)

        def Pv(k, u):
            return PPb[k % 3][:, u, 0, :]

        def PTv(k, u):
            if k == 0:
                return LQB[:, u, 0:128]
            return PPb[k % 3][:, u, 1, :]

        def pkeys(k, hp):
            return [('PPb', k % 3, hp)] + (['LQB'] if k == 0 else [])

        def stage_P(k):
            for u in range(4):
                hp, h2 = u // 2, u % 2
                kb.op('pe', lambda e_, u=u, hp=hp, h2=h2: e_.matmul(psP[hp][:, h2, 0, :], PTv(k - 1, u), Pv(k - 1, u), start=True, stop=True),
                      r=pkeys(k - 1, hp), w=[K_(hp)])
                if k < 6:
                    kb.op('pe', lambda e_, u=u, hp=hp, h2=h2: e_.matmul(psP[hp][:, h2, 1, :], Pv(k - 1, u), PTv(k - 1, u), start=True, stop=True),
                          r=pkeys(k - 1, hp), w=[K_(hp)])
            for hp in range(2):
                eng = 'act' if hp == 0 else 'dve'
                if k < 6:
                    o_, i_ = PPb[k % 3][:, 2 * hp:2 * hp + 2, :, :], psP[hp]
                else:
                    o_, i_ = PPb[k % 3][:, 2 * hp:2 * hp + 2, 0, :], psP[hp][:, :, 0, :]
                if eng == 'act':
                    kb.op('act', lambda e_, o_=o_, i_=i_: e_.activation(out=o_, in_=i_, func=AF.Copy), r=[K_(hp)], w=[('PPb', k % 3, hp)])
                else:
                    kb.op('dve', lambda e_, o_=o_, i_=i_: e_.tensor_copy(out=o_, in_=i_), r=[K_(hp)], w=[('PPb', k % 3, hp)])

        def stage_T(k):
            pv, nx = (k - 1) % 2, k % 2
            for u in range(4):
                hp = u // 2
                kb.op('pe', lambda e_, u=u, hp=hp: e_.matmul(psTh[hp][:, u % 2, :], Pv(k, u), TT[pv][:, u, :], start=True, stop=False),
                      r=[('PPb', k % 3, hp), ('TT', pv, hp)], w=[('psT', hp), K_(2 + hp)])
                kb.op('pe', lambda e_, u=u, hp=hp: e_.matmul(psTh[hp][:, u % 2, :], identb[:], TT[pv][:, u, :], start=False, stop=True),
                      r=['identb', ('TT', pv, hp)], w=[('psT', hp), K_(2 + hp)])
            for hp in range(2):
                us = slice(2 * hp, 2 * hp + 2)
                if (k + hp) % 2 == 0:
                    kb.op('act', lambda e_, us=us, hp=hp: e_.activation(out=TT[nx][:, us, :], in_=psTh[hp], func=AF.Copy),
                          r=[('psT', hp)], w=[('TT', nx, hp)])
                else:
                    kb.op('dve', lambda e_, us=us, hp=hp: e_.tensor_copy(out=TT[nx][:, us, :], in_=psTh[hp]), r=[('psT', hp)], w=[('TT', nx, hp)])

        stage_P(1)
        for k in range(2, 7):
            stage_P(k)
            stage_T(k - 1)
        stage_T(6)
        TTf = TT[0]
        for u in range(4):
            hp, h2 = u // 2, u % 2
            hs = slice(h2 * 64, (h2 + 1) * 64)
            kb.op('pe', lambda e_, u=u, hp=hp, hs=hs: e_.matmul(psW[:, u, :], LQA[:, u, 0:128], tokS[:, hp, 3, hs], start=True, stop=True),
                  r=['LQA', 'tokS'], w=['psW'])
        kb.op('dve', lambda e_: e_.tensor_copy(out=Wsb[:], in_=psW), r=['psW'], w=['Wsb'])
        for u in range(4):
            hp, h2 = u // 2, u % 2
            hs = slice(h2 * 64, (h2 + 1) * 64)
            kb.op('pe', lambda e_, u=u, hp=hp, hs=hs: e_.matmul(psAV[:, u, 0:64], TTf[:, u, :], tokS[:, hp, 0, hs], start=True, stop=True),
                  r=[('TT', 0, 0), ('TT', 0, 1), 'tokS'], w=[K_(3), ('psT', 1)])
            kb.op('pe', lambda e_, u=u: e_.matmul(psAV[:, u, 64:128], TTf[:, u, :], Wsb[:, u, :], start=True, stop=True),
                  r=[('TT', 0, 0), ('TT', 0, 1), 'Wsb'], w=[K_(3), ('psT', 1)])
        kb.op('act', lambda e_: e_.activation(out=AV[:], in_=psAV, func=AF.Copy), r=[K_(3)], w=['AV'])
        for u in range(4):
            hp, h2 = u // 2, u % 2
            hs = slice(h2 * 64, (h2 + 1) * 64)
            kb.op('pe', lambda e_, u=u, hp=hp, hs=hs: e_.matmul(psR[:, u, :], identb[:, hs], pre['r'][:, hp, cs], start=True, stop=False),
                  r=['identb', ('pre', 'r')], w=[K_(6)])
            kb.op('pe', lambda e_, u=u: e_.matmul(psR[:, u, :], AV[:, u, 0:64], LQB[:, u, 128:256], start=False, stop=True),
                  r=['AV', 'LQB'], w=[K_(6)])
        kb.op('dve', lambda e_: e_.tensor_copy(out=RT[:], in_=psR), r=[K_(6)], w=['RT'])
        for u in range(4):
            hp, h2 = u // 2, u % 2
            hs = slice(h2 * 64, (h2 + 1) * 64)
            kb.op('pe', lambda e_, u=u, hp=hp, hs=hs: e_.matmul(psM[:, u, :], identb[:, hs], DG[i][:, hp, hs], start=True, stop=False),
                  r=['identb', ('DG', i)], w=['psW'])
            kb.op('pe', lambda e_, u=u, hp=hp, hs=hs: e_.matmul(psM[:, u, :], AV[:, u, 0:64], tokS[:, hp, 2, hs], start=False, stop=True),
                  r=['AV', 'tokS'], w=['psW'])
        kb.op('act', lambda e_: e_.activation(out=MT[:], in_=psM, func=AF.Copy), r=['psW'], w=['MT'])
        for u in range(4):
            hp, h2 = u // 2, u % 2
            hs = slice(h2 * 64, (h2 + 1) * 64)
            kb.op('pe', lambda e_, u=u, hp=hp, hs=hs: e_.matmul(psY[:, u, :], LQA[:, u, 128:256], tokS[:, hp, 3, hs], start=True, stop=False),
                  r=['LQA', 'tokS'], w=[K_(7)])
            kb.op('pe', lambda e_, u=u: e_.matmul(psY[:, u, :], LQB[:, u, 128:256], AV[:, u, 64:128], start=False, stop=False),
                  r=['LQB', 'AV'], w=[K_(7)])
            kb.op('pe', lambda e_, u=u, cur=cur: e_.matmul(psY[:, u, :], RT[:, u, :], H[cur][:, u, :], start=False, stop=True),
                  r=['RT', ('H', cur)], w=[K_(7)])
        ys = n % 2
        kb.op('dve', lambda e_, ys=ys: e_.tensor_copy(out=yst[ys][:], in_=psY), r=[K_(7)], w=[('ryst', ys)])
        kb.dma('sp', C.Yd[e, c * 128:(c + 1) * 128, :], yst[ys][:].rearrange("p u c -> p (u c)"), r=[('ryst', ys)])
        for u in range(4):
            hp, h2 = u // 2, u % 2
            hs = slice(h2 * 64, (h2 + 1) * 64)
            kb.op('pe', lambda e_, u=u, hp=hp, hs=hs: e_.matmul(psN[:, u, :], tokS[:, hp, 1, hs], tokS[:, hp, 3, hs], start=True, stop=False),
                  r=['tokS'], w=[K_(7)])
            kb.op('pe', lambda e_, u=u, hp=hp, hs=hs: e_.matmul(psN[:, u, :], tokS[:, hp, 2, hs], AV[:, u, 64:128], start=False, stop=False),
                  r=['tokS', 'AV'], w=[K_(7)])
            kb.op('pe', lambda e_, u=u, cur=cur: e_.matmul(psN[:, u, :], MT[:, u, :], H[cur][:, u, :], start=False, stop=True),
                  r=['MT', ('H', cur)], w=[K_(7)])
        kb.op('act', lambda e_, cur=cur: e_.activation(out=H[1 - cur][:], in_=psN, func=AF.Copy), r=[K_(7)], w=[('H', 1 - cur)])

    cur = 0
    for n in range(nchunks):
        chunk_body(n, cur)
        cur = 1 - cur
    kb.end()


def phase_R3(C, l):
    kb = C.kb
    kb.begin()
    ppt = kb.alloc("ppt3", [128, NPP], F32)
    kb.dma('sp', ppt[:], C.pp[l], w=['ppt'])
    blk = kb.alloc("blk3", [128, 128], F32)
    kb.dma('sp', blk[:], C.blk[:, :], w=['blk'])
    g2z = kb.alloc("g2z", [128, G], F32)
    kb.op('dve', lambda e_: e_.memset(g2z[:], 0.0), w=['g2z'])
    kb.dma('sp', g2z[64:128, :], C.rw_g2[l], w=['g2z'])
    ynT = kb.alloc("ynT", [128, 2, T], BF16)
    y0 = [kb.alloc(f"y0{i}", [128, 4, 64], F32) for i in range(2)]
    y1 = [kb.alloc(f"y1{i}", [128, 4, 64], F32) for i in range(2)]
    yc = [kb.alloc(f"ycn{i}", [128, 4, 64], F32) for i in range(2)]
    sq = kb.alloc("sq3", [128, 4, 64], F32)
    st = [kb.alloc(f"st3{i}", [128, 16], F32) for i in range(2)]
    ynb = [kb.alloc(f"ynb{i}", [128, G], BF16) for i in range(2)]
    ptt = [kb.palloc(f"ptt{i}", [128, 2, 128], BF16) for i in range(2)]
    for tt in range(NTT):
        i = tt % 2
        kb.dma('sp', y0[i][:].rearrange("p u c -> p (u c)"), C.Yd[0, tt * 128:(tt + 1) * 128, :], w=[('y0', i)])
        kb.dma('sp', y1[i][:].rearrange("p u c -> p (u c)"), C.Yd[1, tt * 128:(tt + 1) * 128, :], w=[('y1', i)])
        kb.op('pool', lambda e_, i=i: e_.tensor_tensor(out=y0[i][:], in0=y0[i][:], in1=y1[i][:], op=ALU.add),
              r=[('y0', i), ('y1', i)], w=[('y0', i)])
        s = st[i]
        sk = ('st3', i)
        kb.op('dve', lambda e_, i=i, s=s: e_.reduce_sum(out=s[:, 0:4], in_=y0[i][:], axis=AX.X), r=[('y0', i)], w=[sk])
        kb.op('dve', lambda e_, s=s: e_.tensor_scalar(s[:, 4:8], s[:, 0:4], 1.0 / 64, None, ALU.mult), r=[sk], w=[sk])
        kb.op('dve', lambda e_, i=i, s=s: e_.tensor_tensor(out=yc[i][:], in0=y0[i][:], in1=s[:, 4:8].unsqueeze(2).to_broadcast([128, 4, 64]),
                                                           op=ALU.subtract),
              r=[('y0', i), sk], w=[('ycn', i)])
        kb.op('pool', lambda e_, i=i: e_.tensor_tensor(out=sq[:], in0=yc[i][:], in1=yc[i][:], op=ALU.mult), r=[('ycn', i)], w=['sq3'])
        kb.op('dve', lambda e_, s=s: e_.reduce_sum(out=s[:, 8:12], in_=sq[:], axis=AX.X), r=['sq3'], w=[sk])
        kb.op('act', lambda e_, s=s: e_.activation(out=s[:, 12:16], in_=s[:, 8:12], func=AF.Sqrt, bias=C.cst[:, 1:2], scale=1.0 / 64),
              r=[sk, 'cst'], w=[sk])
        kb.op('dve', lambda e_, s=s: e_.reciprocal(s[:, 12:16], s[:, 12:16]), r=[sk], w=[sk])
        kb.op('dve', lambda e_, i=i, s=s: e_.tensor_tensor(out=ynb[i][:].rearrange("p (u c) -> p u c", u=4), in0=yc[i][:],
                                                           in1=s[:, 12:16].unsqueeze(2).to_broadcast([128, 4, 64]), op=ALU.mult),
              r=[('ycn', i), sk], w=[('ynb', i)])
        for hp in range(2):
            kb.op('pe', lambda e_, i=i, hp=hp: e_.transpose(ptt[i][:, hp, :], ynb[i][:, hp * 128:(hp + 1) * 128], C.identb[:]),
                  r=[('ynb', i), 'identb'], w=[('ptt', i)])
        kb.op('act', lambda e_, i=i, tt=tt: e_.activation(out=ynT[:, :, tt * 128:(tt + 1) * 128], in_=ptt[i][:], func=AF.Copy),
              r=[('ptt', i)], w=[('ynT', tt // 4)])
    z6 = [kb.alloc(f"z6{i}", [128, 512], F32) for i in range(2)]
    sgl = [kb.alloc(f"sgl{i}", [128, 512], F32) for i in range(2)]
    zr = [kb.alloc(f"zr3{i}", [128, 512], F32) for i in range(2)]
    zk = [kb.alloc(f"zk3{i}", [128, 512], F32) for i in range(2)]
    zv = [kb.alloc(f"zv3{i}", [128, 512], F32) for i in range(2)]
    rk = [kb.alloc(f"rk3{i}", [128, 512], F32) for i in range(2)]
    o1 = [kb.alloc(f"o13{i}", [128, 512], F32) for i in range(2)]
    bon = [kb.alloc(f"bon3{i}", [128, 512], F32) for i in range(2)]
    yo = kb.alloc("yo3", [128, 2, T], BF16)
    pbs = [kb.palloc(f"pbs{i}", [128, 512], F32) for i in range(2)]
    pg = [kb.palloc(f"pg{i}", [128, 512], F32) for i in range(2)]
    n = 0
    for g in range(8):
        gs = slice(g * 512, (g + 1) * 512)
        j = g % 2
        kb.dma('sp', z6[j][:], C.zT_rw[768:896, gs], w=[('z6', j)])
        kb.op('act', lambda e_, j=j: e_.activation(out=sgl[j][:], in_=z6[j][:], func=AF.Sigmoid), r=[('z6', j)], w=[('sgl', j)])
        for hp in range(2):
            i = n % 2
            n += 1
            kb.dma('sp', zr[i][:], C.zT_rw[hp * 128:(hp + 1) * 128, gs], w=[('zr3', i)])
            kb.dma('sp', zk[i][:], C.zT_rw[256 + hp * 128:256 + (hp + 1) * 128, gs], w=[('zk3', i)])
            kb.dma('sp', zv[i][:], C.zT_rw[512 + hp * 128:512 + (hp + 1) * 128, gs], w=[('zv3', i)])
            rkc = PP['r_k'] + hp
            kb.op('dve', lambda e_, i=i, rkc=rkc: e_.scalar_tensor_tensor(out=rk[i][:], in0=zr[i][:], scalar=ppt[:, rkc:rkc + 1], in1=zk[i][:],
                                                                            op0=ALU.mult, op1=ALU.mult),
                  r=[('zr3', i), ('zk3', i), 'ppt'], w=[('rk3', i)])
            kb.op('pe', lambda e_, i=i: e_.matmul(pbs[i][:], blk[:], rk[i][:], start=True, stop=True), r=['blk', ('rk3', i)], w=[('pbs', i)])
            kb.op('pe', lambda e_, i=i, j=j, hp=hp: e_.matmul(pg[i][:], g2z[:, hp * 128:(hp + 1) * 128], sgl[j][:], start=True, stop=True),
                  r=['g2z', ('sgl', j)], w=[('pg', i)])
            lw_, lb_ = PP['lnx_w'] + hp, PP['lnx_b'] + hp
            kb.op('dve', lambda e_, i=i, hp=hp, gs=gs, lw_=lw_, lb_=lb_: e_.tensor_scalar(
                o1[i][:], ynT[:, hp, gs], ppt[:, lw_:lw_ + 1], ppt[:, lb_:lb_ + 1], ALU.mult, ALU.add),
                r=[('ynT', g), 'ppt'], w=[('o13', i)])
            kb.op('dve', lambda e_, i=i: e_.tensor_tensor(out=bon[i][:], in0=pbs[i][:], in1=zv[i][:], op=ALU.mult),
                  r=[('pbs', i), ('zv3', i)], w=[('bon3', i)])
            kb.op('pool', lambda e_, i=i: e_.tensor_tensor(out=o1[i][:], in0=o1[i][:], in1=bon[i][:], op=ALU.add),
                  r=[('o13', i), ('bon3', i)], w=[('o13', i)])
            kb.op('dve', lambda e_, i=i, hp=hp, gs=gs: e_.tensor_tensor(out=yo[:, hp, gs], in0=pg[i][:], in1=o1[i][:], op=ALU.mult),
                  r=[('pg', i), ('o13', i)], w=[('yo3', hp)])
    for hp in range(2):
        kb.dma('sp', C.yT[256 + hp * 128:256 + (hp + 1) * 128, :], yo[:, hp, :], r=[('yo3', hp)])
    kb.end()


def build_program():
    nc = bass.Bass("TRN2", target_bir_lowering=False)
    C = declare(nc)
    declare2(C, nc)
    declare3(C, nc)
    declare4(C, nc)
    declare5(C, nc)
    declare6(C, nc)
    with ExitStack() as st:
        alloc_persistent(C, st)
        alloc_persistent2(C, st)
        phase_init(C)
        for l in range(2):
            xsrc = C.x if l == 0 else C.out
            moe = (l == 1)
            phase_mod(C, l)
            phase_A(C, l, xsrc)
            phase_NA(C, l)
            for e in range(2):
                phase_R1(C, l, e)
            for e in range(2):
                phase_R2(C, l, e)
            phase_R3(C, l)
            phase_DE(C, l)
            phase_F(C, l, xsrc, moe)
            phase_FFN(C, l, moe)
    return nc


def kernel(**inp):
    inp = {k: np.asarray(v) for k, v in inp.items()}
    B = inp["x"].shape[0]
    shared = {
        "ident": np.eye(128, dtype=np.float32),
        "pp": host_pack_pp(inp),
        "pool_rc": host_pool_rc(),
        "pool_w": np.ascontiguousarray(inp["pool_w"], dtype=np.float32),
        "router_wT": np.ascontiguousarray(inp["router_w"][0].T),
        "na_tab": np.stack([host_na_table(inp["na_rpb"][l]) for l in range(2)]),
        "rw_masks": host_rw_masks(),
        "blk": host_blk(),
        "ffn_w1": np.ascontiguousarray(inp["ffn_w1"][0]),
        "ffn_w3": np.ascontiguousarray(inp["ffn_w3"][0]),
        "ffn_w2": np.ascontiguousarray(inp["ffn_w2"][0]),
        "moe_w1": np.ascontiguousarray(inp["moe_w1"][0]),
        "moe_w3": np.ascontiguousarray(inp["moe_w3"][0]),
        "moe_w2": np.ascontiguousarray(inp["moe_w2"][0]),
    }
    for k in ["ada_w", "ada_b", "norm_g", "w_in", "w_out", "rw_w2", "rw_a2", "rw_g2"]:
        shared[k] = np.ascontiguousarray(inp[k], dtype=np.float32)
    in_maps = []
    for b in range(B):
        m = dict(shared)
        m["x"] = np.ascontiguousarray(inp["x"][b], dtype=np.float32)
        m["c_t"] = np.ascontiguousarray(inp["c"][b].reshape(8, 128).T, dtype=np.float32)
        in_maps.append(m)
    nc = build_program()
    res = run_bass_kernel_spmd(nc, in_maps, core_ids=list(range(B)))
    return np.stack([np.asarray(r["out"], dtype=np.float32) for r in res.results], axis=0)
```
